# Optimizing a Trainium2 kernel written in Bass

```python
import jax, jax.numpy as jnp
from jax import lax
import numpy as np

D_MODEL = 1024
BATCH = 16
SEQ = 2048
DEPTH = 4

CHUNK = 64
HEAD_DIM = 64
SWA_HEADS = 8
SWA_KV_HEADS = 2
SWA_GROUP = SWA_HEADS // SWA_KV_HEADS
SWA_WINDOW = 128
WINDOW_CHUNKS = SWA_WINDOW // CHUNK
SPAN = (WINDOW_CHUNKS + 1) * CHUNK
CONV_WIDTH = 256
CONV_K = 3
MEM_HEADS = 4
MEM_LEN = 256
SWA_WIDTH = SWA_HEADS * HEAD_DIM
KV_WIDTH = SWA_KV_HEADS * HEAD_DIM
MEM_WIDTH = MEM_HEADS * HEAD_DIM
MIX_WIDTH = SWA_WIDTH + CONV_WIDTH + MEM_WIDTH
IN_WIDTH = SWA_WIDTH + 2 * KV_WIDTH + 3 * CONV_WIDTH + MEM_WIDTH
SPLIT_POINTS = (SWA_WIDTH,
                SWA_WIDTH + KV_WIDTH,
                SWA_WIDTH + 2 * KV_WIDTH,
                SWA_WIDTH + 2 * KV_WIDTH + CONV_WIDTH,
                SWA_WIDTH + 2 * KV_WIDTH + 2 * CONV_WIDTH,
                SWA_WIDTH + 2 * KV_WIDTH + 3 * CONV_WIDTH)
D_FF = 3584
N_EXPERTS = 8
TOP_K = 2
N_DENSE = (DEPTH + 1) // 2
N_MOE = DEPTH // 2
EPS = 1e-6

kernel_name = 'hymba_style_chunk_causal_swa_conv_mem_moe_trunk'


def rms_norm(x, g):
    xf = x.astype(jnp.float32)
    y = xf * lax.rsqrt(jnp.mean(xf * xf, axis=-1, keepdims=True) + EPS)
    return (y * g.astype(jnp.float32)).astype(x.dtype)


def alibi_slopes(n):
    return jnp.asarray([2.0 ** (-8.0 * (i + 1) / n) for i in range(n)], dtype=jnp.float32)


def swa_sink_attention(q, k, v, sinks):
    b, s = q.shape[0], q.shape[1]
    nc = s // CHUNK
    qc = q.reshape(b, nc, CHUNK, SWA_KV_HEADS, SWA_GROUP, HEAD_DIM)
    pad = ((0, 0), (WINDOW_CHUNKS * CHUNK, 0), (0, 0), (0, 0))

    def band(t):
        tp = jnp.pad(t, pad).reshape(b, nc + WINDOW_CHUNKS, CHUNK, SWA_KV_HEADS, HEAD_DIM)
        return jnp.concatenate([tp[:, j:j + nc] for j in range(WINDOW_CHUNKS + 1)], axis=2)

    kb, vb = band(k), band(v)
    scores = jnp.einsum('bnqkgd,bnskd->bnkgqs', qc, kb).astype(jnp.float32) * (HEAD_DIM ** -0.5)
    qi = jnp.arange(CHUNK)[:, None] + WINDOW_CHUNKS * CHUNK
    sj = jnp.arange(SPAN)[None, :]
    dist = jnp.abs(qi - sj).astype(jnp.float32)
    slopes = alibi_slopes(SWA_HEADS).reshape(SWA_KV_HEADS, SWA_GROUP)
    scores = scores - slopes[None, None, :, :, None, None] * dist[None, None, None, None]
    key_chunk = jnp.arange(nc)[:, None] - WINDOW_CHUNKS + (jnp.arange(SPAN) // CHUNK)[None, :]
    valid = key_chunk >= 0
    scores = jnp.where(valid[None, :, None, None, None, :], scores, -jnp.inf)
    sink = sinks.astype(jnp.float32).reshape(SWA_KV_HEADS, SWA_GROUP)[None, None, :, :, None, None]
    m = jnp.maximum(jnp.max(scores, axis=-1, keepdims=True), sink)
    p = jnp.exp(scores - m)
    denom = jnp.sum(p, axis=-1, keepdims=True) + jnp.exp(sink - m)
    p = (p / denom).astype(v.dtype)
    out = jnp.einsum('bnkgqs,bnskd->bnqkgd', p, vb)
    return out.reshape(b, s, SWA_WIDTH)


def gated_short_conv(gate_b, gate_c, u, w):
    s = u.shape[1]
    z = gate_c * u
    zp = jnp.pad(z, ((0, 0), (CONV_K - 1, 0), (0, 0)))
    y = w[0] * zp[:, 0:s]
    for j in range(1, CONV_K):
        y = y + w[j] * zp[:, j:j + s]
    return gate_b * y


def memory_attention(q, mk, mv):
    b, s = q.shape[0], q.shape[1]
    scores = jnp.einsum('bshd,bmhd->bhsm', q, mk).astype(jnp.float32) * (HEAD_DIM ** -0.5)
    p = jax.nn.softmax(scores, axis=-1).astype(mv.dtype)
    out = jnp.einsum('bhsm,bmhd->bshd', p, mv)
    return out.reshape(b, s, MEM_WIDTH)


def swiglu(h, wg, wu, wd):
    return (jax.nn.silu(h @ wg) * (h @ wu)) @ wd


def moe_ffn(h, w_router, b_router, wg, wu, wd):
    b, s, d = h.shape
    t = h.reshape(b * s, d)
    logits = (t @ w_router).astype(jnp.float32) + b_router.astype(jnp.float32)
    top_val, top_idx = lax.top_k(logits, TOP_K)
    gates = jax.nn.softmax(top_val, axis=-1)
    combine = jnp.sum(jax.nn.one_hot(top_idx, N_EXPERTS, dtype=jnp.float32) * gates[..., None], axis=1)
    combine = combine.astype(t.dtype)
    out = jnp.zeros_like(t)
    for e in range(N_EXPERTS):
        out = out + combine[:, e:e + 1] * swiglu(t, wg[e], wu[e], wd[e])
    return out.reshape(b, s, d)


def setup_inputs(seed: int = 0) -> dict:
    key = jax.random.key(seed)
    ks = jax.random.split(key, 32)
    f32 = jnp.float32
    res = (2 * DEPTH) ** -0.5

    def nrm(k, shape, scale):
        return jax.random.normal(k, shape, f32) * scale

    def gain(k, shape):
        return 1.0 + 0.1 * jax.random.normal(k, shape, f32)

    return {
        'x': nrm(ks[0], (BATCH, SEQ, D_MODEL), 1.0),
        'mem': nrm(ks[1], (BATCH, MEM_LEN, D_MODEL), 1.0),
        'g_mix': gain(ks[2], (DEPTH, D_MODEL)),
        'w_in': nrm(ks[3], (DEPTH, D_MODEL, IN_WIDTH), D_MODEL ** -0.5),
        'g_q_swa': gain(ks[4], (DEPTH, HEAD_DIM)),
        'g_k_swa': gain(ks[5], (DEPTH, HEAD_DIM)),
        'sinks': nrm(ks[6], (DEPTH, SWA_HEADS), 1.0),
        'conv_w': nrm(ks[7], (DEPTH, CONV_K, CONV_WIDTH), CONV_K ** -0.5),
        'g_mem': gain(ks[8], (DEPTH, D_MODEL)),
        'w_mem_kv': nrm(ks[9], (DEPTH, D_MODEL, 2 * MEM_WIDTH), D_MODEL ** -0.5),
        'g_q_mem': gain(ks[10], (DEPTH, HEAD_DIM)),
        'g_k_mem': gain(ks[11], (DEPTH, HEAD_DIM)),
        'g_out_swa': gain(ks[12], (DEPTH, SWA_WIDTH)),
        'g_out_conv': gain(ks[13], (DEPTH, CONV_WIDTH)),
        'g_out_mem': gain(ks[14], (DEPTH, MEM_WIDTH)),
        'w_out': nrm(ks[15], (DEPTH, MIX_WIDTH, D_MODEL), MIX_WIDTH ** -0.5 * res),
        'g_ffn': gain(ks[16], (DEPTH, D_MODEL)),
        'w_gate_dense': nrm(ks[17], (N_DENSE, D_MODEL, D_FF), D_MODEL ** -0.5),
        'w_up_dense': nrm(ks[18], (N_DENSE, D_MODEL, D_FF), D_MODEL ** -0.5),
        'w_down_dense': nrm(ks[19], (N_DENSE, D_FF, D_MODEL), D_FF ** -0.5 * res),
        'w_router': nrm(ks[20], (N_MOE, D_MODEL, N_EXPERTS), D_MODEL ** -0.5),
        'b_router': nrm(ks[21], (N_MOE, N_EXPERTS), 0.01),
        'w_gate_moe': nrm(ks[22], (N_MOE, N_EXPERTS, D_MODEL, D_FF), D_MODEL ** -0.5),
        'w_up_moe': nrm(ks[23], (N_MOE, N_EXPERTS, D_MODEL, D_FF), D_MODEL ** -0.5),
        'w_down_moe': nrm(ks[24], (N_MOE, N_EXPERTS, D_FF, D_MODEL), D_FF ** -0.5 * res),
    }


def reference(x, mem, g_mix, w_in, g_q_swa, g_k_swa, sinks, conv_w, g_mem, w_mem_kv,
              g_q_mem, g_k_mem, g_out_swa, g_out_conv, g_out_mem, w_out, g_ffn,
              w_gate_dense, w_up_dense, w_down_dense, w_router, b_router,
              w_gate_moe, w_up_moe, w_down_moe):
    b, s, _ = x.shape
    mlen = mem.shape[1]
    for l in range(DEPTH):
        h = rms_norm(x, g_mix[l])
        proj = h @ w_in[l]
        q_s, k_s, v_s, c_b, c_c, c_u, q_m = jnp.split(proj, SPLIT_POINTS, axis=-1)
        q_s = rms_norm(q_s.reshape(b, s, SWA_HEADS, HEAD_DIM), g_q_swa[l])
        k_s = rms_norm(k_s.reshape(b, s, SWA_KV_HEADS, HEAD_DIM), g_k_swa[l])
        v_s = v_s.reshape(b, s, SWA_KV_HEADS, HEAD_DIM)
        y_swa = swa_sink_attention(q_s, k_s, v_s, sinks[l])

        y_conv = gated_short_conv(c_b, c_c, c_u, conv_w[l])

        hm = rms_norm(mem, g_mem[l])
        mk, mv = jnp.split(hm @ w_mem_kv[l], 2, axis=-1)
        mk = rms_norm(mk.reshape(b, mlen, MEM_HEADS, HEAD_DIM), g_k_mem[l])
        mv = mv.reshape(b, mlen, MEM_HEADS, HEAD_DIM)
        q_m = rms_norm(q_m.reshape(b, s, MEM_HEADS, HEAD_DIM), g_q_mem[l])
        y_mem = memory_attention(q_m, mk, mv)

        y = jnp.concatenate([rms_norm(y_swa, g_out_swa[l]),
                             rms_norm(y_conv, g_out_conv[l]),
                             rms_norm(y_mem, g_out_mem[l])], axis=-1)
        x = x + y @ w_out[l]

        h2 = rms_norm(x, g_ffn[l])
        i = l // 2
        if l % 2 == 0:
            x = x + swiglu(h2, w_gate_dense[i], w_up_dense[i], w_down_dense[i])
        else:
            x = x + moe_ffn(h2, w_router[i], b_router[i], w_gate_moe[i], w_up_moe[i], w_down_moe[i])
    return x
```

```python
import contextlib
import numpy as np
import concourse.bass as bass
import concourse.mybir as mybir
from concourse.bass_utils import run_bass_kernel_spmd

F32 = mybir.dt.float32
BF16 = mybir.dt.bfloat16
AF = mybir.ActivationFunctionType
ALU = mybir.AluOpType
AX = mybir.AxisListType

D_MODEL = 1024
SEQ = 2048
NT = SEQ // 128
DEPTH = 4
D_FF = 3584
N_EXP = 8
MEM_LEN = 256
EPS = 1e-6
GW = 512
NG = D_FF // GW
NWB = 3

ENGS = ['pe', 'act', 'dve', 'pool', 'sp']
DEBUG_STAGE = None
DEBUG_LEVEL = 99


class Op:
    __slots__ = ('eng', 'emit', 'deps', 'is_dma', 'dsem', 'dval', 'sig', 'cnt', 'idx')


class Sched:
    def __init__(self, nc):
        self.nc = nc
        self.ops = []
        self.per_eng = {e: [] for e in ENGS}
        self.last_w = {}
        self.readers = {}
        self.dma_val = {}
        self.pending_bar = {}

    def add(self, eng, emit, reads=(), writes=(), dma=False, dsem=None):
        op = Op()
        op.eng = eng; op.emit = emit; op.is_dma = dma; op.idx = len(self.ops)
        op.sig = False; op.cnt = 0; op.dsem = None; op.dval = 0
        deps = set()
        for r in reads:
            w = self.last_w.get(r)
            if w is not None:
                deps.add(w)
        for r in writes:
            w = self.last_w.get(r)
            if w is not None:
                deps.add(w)
            for rd in self.readers.get(r, ()):
                deps.add(rd)
        pb = self.pending_bar.pop(eng, None)
        if pb:
            deps |= pb
        for r in reads:
            self.readers.setdefault(r, []).append(op.idx)
        for r in writes:
            self.last_w[r] = op.idx
            self.readers[r] = []
        if dma:
            v = self.dma_val.get(dsem, 0) + 16
            self.dma_val[dsem] = v
            op.dsem = dsem; op.dval = v
        op.deps = deps
        self.ops.append(op)
        self.per_eng[eng].append(op)
        return op

    def barrier(self):
        last = set()
        for e in ENGS:
            if self.per_eng[e]:
                last.add(self.per_eng[e][-1].idx)
        for e in ENGS:
            self.pending_bar[e] = set(last)

    def finalize(self):
        ops = self.ops
        for op in ops:
            for d in op.deps:
                dop = ops[d]
                if dop.is_dma:
                    continue
                if dop.eng == 'pe' and op.eng == 'pe' and not op.is_dma:
                    continue
                dop.sig = True
        for e in ENGS:
            c = 0
            for op in self.per_eng[e]:
                if op.sig and not op.is_dma:
                    c += 1
                op.cnt = c

    def emit_all(self, final_waits_eng='sp'):
        nc = self.nc
        self.finalize()
        with contextlib.ExitStack() as st:
            esem = {e: st.enter_context(nc.semaphore('s_' + e)) for e in ENGS}
            dsem = {k: st.enter_context(nc.semaphore('d_%d' % i)) for i, k in enumerate(self.dma_val)}
            block = st.enter_context(nc.Block())
            ops = self.ops

            def run(e, eng):
                waited = {}
                for op in self.per_eng[e]:
                    for d in sorted(op.deps):
                        dop = ops[d]
                        if dop.is_dma:
                            key = ('d', dop.dsem); sem = dsem[dop.dsem]; val = dop.dval
                        else:
                            if dop.eng == 'pe' and e == 'pe' and not op.is_dma:
                                continue
                            key = ('e', dop.eng); sem = esem[dop.eng]; val = dop.cnt
                        if waited.get(key, 0) >= val:
                            continue
                        waited[key] = val
                        eng.wait_ge(sem, val)
                    ins = op.emit(eng)
                    if op.is_dma:
                        ins.then_inc(dsem[op.dsem], 16)
                    elif op.sig:
                        ins.then_inc(esem[e], 1)
                if e == final_waits_eng:
                    for k, v in self.dma_val.items():
                        if waited.get(('d', k), 0) < v:
                            eng.wait_ge(dsem[k], v)

            @block.tensor
            def _(eng):
                run('pe', eng)

            @block.scalar
            def _(eng):
                run('act', eng)

            @block.vector
            def _(eng):
                run('dve', eng)

            @block.gpsimd
            def _(eng):
                run('pool', eng)

            @block.sync
            def _(eng):
                run('sp', eng)


GC_MIX, GC_FFN, GC_MEM, GC_OUT, GC_QK, GC_CONV = 0, 32, 64, 96, 128, 160
GC_N = 184
BR_SINK, BR_BR = 0, 32
BR_N = 48


def build_program(layers=(0, 1, 2, 3), n_seq=2):
    nc = bass.Bass("TRN2", target_bir_lowering=False)

    def din(name, shape):
        return nc.dram_tensor(name, list(shape), F32, kind="ExternalInput").ap()

    x_d = din("x", [n_seq, SEQ, D_MODEL])
    mem_d = din("mem", [n_seq, MEM_LEN, D_MODEL])
    w_in_d = din("w_in", [DEPTH, D_MODEL, 1792])
    w_mem_d = din("w_mem_kv", [DEPTH, D_MODEL, 512])
    w_out_d = din("w_out", [DEPTH, D_MODEL, D_MODEL])
    wgd_d = din("w_gate_dense", [2, D_MODEL, D_FF])
    wud_d = din("w_up_dense", [2, D_MODEL, D_FF])
    wdd_d = din("w_down_dense", [2, D_FF, D_MODEL])
    wgm_d = din("w_gate_moe", [2, N_EXP, D_MODEL, D_FF])
    wum_d = din("w_up_moe", [2, N_EXP, D_MODEL, D_FF])
    wdm_d = din("w_down_moe", [2, N_EXP, D_FF, D_MODEL])
    gvec_d = din("gvec", [128, GC_N])
    brow_d = din("brow", [128, BR_N])
    wr_d = din("wr", [128, 128])
    et_d = din("et", [128, 2 * 8 * 128])
    ident_d = din("ident", [128, 128])
    out_d = nc.dram_tensor("out", [n_seq, SEQ, D_MODEL], F32, kind="ExternalOutput").ap()

    S = Sched(nc)
    with contextlib.ExitStack() as st:
        def sb(name, shape, dt):
            return st.enter_context(nc.sbuf_tensor(name, shape, dt))

        X = sb("X", [128, NT, D_MODEL], F32)
        gvec = sb("gvec_s", [128, GC_N], F32)
        brow = sb("brow_s", [128, BR_N], F32)
        esink = sb("esink", [128, 32], F32)
        wr = sb("wr_s", [128, 2, 8, 8], F32)
        ET = sb("ET", [128, 2, 8, 128], F32)
        ident = sb("ident_s", [128, 128], F32)
        identb = sb("identb", [128, 128], BF16)
        ones = sb("ones_s", [128, 128], F32)
        mhalf = sb("mhalf", [128, 128], F32)
        small = sb("small", [128, 256], F32)
        UN = 65 * 1024
        U = sb("U", [128, UN], BF16)
        PB = [st.enter_context(nc.psum_tensor("pb%d" % k, [128, 512], F32)) for k in range(8)]

        class Carver:
            def __init__(self):
                self.off = 0

            def take(self, shape, dt):
                n = int(np.prod(shape[1:]))
                nb = n * (4 if dt == F32 else 2)
                nb = (nb + 63) // 64 * 64
                a = U[:, self.off // 2:(self.off + nb) // 2]
                self.off += nb
                assert self.off <= UN * 2, (self.off, UN * 2)
                if dt == F32:
                    a = a.bitcast(F32)
                a = a[:, 0:n]
                if len(shape) == 3:
                    a = a.rearrange("p (a b) -> p a b", a=shape[1])
                elif len(shape) == 4:
                    a = a.rearrange("p (a b c) -> p a b c", a=shape[1], b=shape[2])
                return a

        cm = Carver()
        Win = cm.take([128, 8, 1792], BF16)
        Wout = cm.take([128, 8, 1024], BF16)
        Wmem = cm.take([128, 8, 512], BF16)
        memx = cm.take([128, 2, 1024], F32)
        hmT = cm.take([128, 8, 256], BF16)
        mkT = cm.take([128, 2, 256], BF16)
        mvA = cm.take([128, 2, 4, 65], BF16)
        mks = cm.take([128, 256], F32)
        mksq = cm.take([128, 256], F32)
        mkn = cm.take([128, 256], BF16)
        xn_m = cm.take([128, 1024], F32)
        junk_m = cm.take([128, 1024], BF16)
        hTt = [cm.take([128, 8, 128], BF16) for _ in range(2)]
        qk = cm.take([128, 896], F32)
        qsq = cm.take([128, 896], F32)
        qkn = cm.take([128, 896], BF16)
        qTt = [cm.take([128, 7, 128], BF16) for _ in range(2)]
        Vaug = [cm.take([128, 2, 65], BF16) for _ in range(2)]
        kz = [[cm.take([128, 128], BF16) for _ in range(2)] for _ in range(2)]
        mkz = cm.take([128, 4, 256], BF16)
        es = [cm.take([128, 512], F32) for _ in range(2)]
        PT = cm.take([128, 4, 512], BF16)
        PmT = cm.take([128, 2, 512], BF16)
        ysw = cm.take([128, 512], F32)
        ym = cm.take([128, 256], F32)
        ynb = cm.take([128, 768], BF16)
        yT = cm.take([128, 8, 128], BF16)
        ccs = cm.take([128, 2, 128], F32)
        zt = [cm.take([128, 2, 130], F32) for _ in range(2)]
        cacc = cm.take([128, 2, 128], F32)
        ycv = cm.take([128, 2, 128], F32)
        csq = cm.take([128, 2, 128], F32)
        rcv = cm.take([128, 128], F32)
        mixer_bytes = cm.off
        cf = Carver()
        hT = cf.take([128, 8, SEQ], BF16)
        Wb = [(cf.take([128, 8, GW], BF16), cf.take([128, 8, GW], BF16), cf.take([128, GW // 128, 1024], BF16))
              for _ in range(NWB)]
        AT = [cf.take([128, GW // 128, 512], BF16) for _ in range(2)]
        sg = [cf.take([128, 512], BF16) for _ in range(2)]
        xn_f = cf.take([128, 1024], F32)
        junk_f = cf.take([128, 1024], BF16)
        h2f = cf.take([128, 8, 128], F32)
        L = cf.take([128, NT, 8], F32)
        L2 = cf.take([128, NT, 8], F32)
        eq1 = cf.take([128, NT, 8], F32)
        eq2 = cf.take([128, NT, 8], F32)
        comb = cf.take([128, NT, 8], F32)
        ffn_bytes = cf.off

        sm_off = [0]

        def smalloc(n):
            a = small[:, sm_off[0]:sm_off[0] + n]
            sm_off[0] += n
            assert sm_off[0] <= 256
            return a

        ss_ = [smalloc(1) for _ in range(2)]
        ss2_ = [smalloc(1) for _ in range(2)]
        rinv_ = [smalloc(1) for _ in range(2)]
        ssq = smalloc(14); ssq2 = smalloc(14); rq = smalloc(14)
        mss = smalloc(4); mss2 = smalloc(4); mrq = smalloc(4)
        den = smalloc(8); rden = smalloc(8)
        denm = smalloc(4); rdm = smalloc(4)
        oss = smalloc(2); oss2 = smalloc(2); orin = smalloc(2)
        oscale = smalloc(2)
        m1 = smalloc(16); m2 = smalloc(16); dd = smalloc(16); ee = smalloc(16); g1 = smalloc(16); g2 = smalloc(16)

        add = S.add
        nstat = [0]

        add('sp', lambda e: e.dma_start(out=gvec[:], in_=gvec_d), writes=['gvec'], dma=True, dsem='gvec')
        add('sp', lambda e: e.dma_start(out=brow[:], in_=brow_d), writes=['brow'], dma=True, dsem='brow')
        add('sp', lambda e: e.dma_start(out=wr[:], in_=wr_d.rearrange("p (m k e) -> p m k e", m=2, k=8)),
            writes=['wr'], dma=True, dsem='wr')
        add('sp', lambda e: e.dma_start(out=ET[:], in_=et_d.rearrange("p (r h q) -> p r h q", r=2, h=8)),
            writes=['ET'], dma=True, dsem='ET')
        add('sp', lambda e: e.dma_start(out=ident[:], in_=ident_d), writes=['ident'], dma=True, dsem='ident')
        add('pool', lambda e: e.memset(mhalf[:], -0.5), writes=['mhalf'])
        add('pool', lambda e: e.memset(ones[:], 1.0), writes=['ones'])
        add('pool', lambda e: e.memset(oscale[:, 0:1], 1.0 / 512), writes=['oscale'])
        add('pool', lambda e: e.memset(oscale[:, 1:2], 1.0 / 256), writes=['oscale'])
        add('act', lambda e: e.activation(out=identb[:], in_=ident[:], func=AF.Copy), reads=['ident'], writes=['identb'])
        add('act', lambda e: e.activation(out=esink[:], in_=brow[:, BR_SINK:BR_SINK + 32], func=AF.Exp),
            reads=['brow'], writes=['esink'])

        def norm_T(src, src_res, gcol, dst, dst_res, banks, xn, junk, tag, f32dst=None, f32res=None):
            k = nstat[0] % 2
            nstat[0] += 1
            ss, ss2, rinv = ss_[k], ss2_[k], rinv_[k]
            add('act', lambda e: e.activation(out=junk, in_=src, func=AF.Square, accum_out=ss),
                reads=[src_res], writes=[(tag, 'junk'), ('ss', k)])
            add('dve', lambda e: e.tensor_scalar(out=ss2, in0=ss, scalar1=1.0 / D_MODEL, scalar2=EPS,
                                                 op0=ALU.mult, op1=ALU.add), reads=[('ss', k)], writes=[('ss2', k)])
            add('pool', lambda e: e.tensor_tensor(out=rinv, in0=ss2, in1=mhalf[:, 0:1], op=ALU.pow),
                reads=[('ss2', k), 'mhalf'], writes=[('rinv', k)])
            add('act', lambda e: e.activation(out=xn, in_=src, func=AF.Copy, scale=rinv),
                reads=[src_res, ('rinv', k)], writes=[(tag, 'xn')])
            for kc in range(8):
                b = banks[kc // 4]
                add('pe', lambda e, kc=kc, b=b: e.transpose(out=PB[b][:, (kc % 4) * 128:(kc % 4 + 1) * 128],
                                                            in_=xn[:, kc * 128:(kc + 1) * 128], identity=ident[:]),
                    reads=[(tag, 'xn'), 'ident'], writes=[('pb', b)])
            for j in range(2):
                b = banks[j]
                pin = PB[b][:].rearrange("p (a b) -> p a b", a=4)
                gin = gcol[:, 4 * j:4 * j + 4].unsqueeze(2).broadcast_to([128, 4, 128])
                if f32dst is not None:
                    add('dve', lambda e, j=j, pin=pin, gin=gin: e.tensor_tensor(out=f32dst[:, 4 * j:4 * j + 4, :], in0=pin, in1=gin, op=ALU.mult),
                        reads=[('pb', b), 'gvec'], writes=[f32res])
                add('dve', lambda e, j=j, pin=pin, gin=gin: e.tensor_tensor(out=dst[:, 4 * j:4 * j + 4, :], in0=pin, in1=gin, op=ALU.mult),
                    reads=[('pb', b), 'gvec'], writes=[dst_res])

        def mixer(s, l):
            add('pool', lambda e: e.dma_start(out=Wmem, in_=w_mem_d[l].rearrange("(kc p) n -> p kc n", p=128)),
                writes=['Wmem'], dma=True, dsem='Wmem')
            add('pool', lambda e: e.dma_start(out=Win, in_=w_in_d[l].rearrange("(kc p) n -> p kc n", p=128)),
                writes=['Win'], dma=True, dsem='Win')
            add('pool', lambda e: e.dma_start(out=Wout, in_=w_out_d[l].rearrange("(kc p) n -> p kc n", p=128)),
                writes=['Wout'], dma=True, dsem='Wout')
            add('sp', lambda e: e.dma_start(out=memx, in_=mem_d[s].rearrange("(t p) d -> p t d", p=128)),
                writes=['memx'], dma=True, dsem='memx')
            add('pool', lambda e: e.memset(mvA[:, :, :, 64:65], 1.0), writes=['mvA'])
            for k in range(2):
                add('pool', lambda e, k=k: e.memset(Vaug[k][:, :, 64:65], 1.0), writes=[('Vaug', k)])
            add('pool', lambda e: e.memset(zt[0][:, :, 0:2], 0.0), writes=[('zt', 0)])
            for k in range(2):
                for kv in range(2):
                    add('pool', lambda e, k=k, kv=kv: e.memset(kz[k][kv][:], 0.0), writes=[('kz', k)])
            add('pool', lambda e: e.memset(mkz[:], 0.0), writes=['mkz'])

            for mt in range(2):
                norm_T(memx[:, mt, :], 'memx', gvec[:, GC_MEM + l * 8:GC_MEM + l * 8 + 8],
                       hmT[:, :, mt * 128:(mt + 1) * 128], 'hmT', (0, 1), xn_m, junk_m, 'm')
                for kc in range(8):
                    add('pe', lambda e, kc=kc, mt=mt: e.matmul(PB[2][:], lhsT=hmT[:, kc, mt * 128:(mt + 1) * 128], rhs=Wmem[:, kc, :],
                                                               start=(kc == 0), stop=(kc == 7)),
                        reads=['hmT', 'Wmem'], writes=[('pb', 2)])
                add('act', lambda e: e.activation(out=mks, in_=PB[2][:, 0:256], func=AF.Copy), reads=[('pb', 2)], writes=['mks'])
                add('act', lambda e, mt=mt: e.activation(out=mvA[:, mt, :, 0:64], in_=PB[2][:, 256:512].rearrange("p (h d) -> p h d", h=4), func=AF.Copy),
                    reads=[('pb', 2)], writes=['mvA'])
                add('dve', lambda e: e.tensor_tensor(out=mksq, in0=mks, in1=mks, op=ALU.mult), reads=['mks'], writes=['mksq'])
                add('dve', lambda e: e.reduce_sum(out=mss, in_=mksq.rearrange("p (h d) -> p h d", h=4), axis=AX.X), reads=['mksq'], writes=['mss'])
                add('dve', lambda e: e.tensor_scalar(out=mss2, in0=mss, scalar1=1.0 / 64, scalar2=EPS, op0=ALU.mult, op1=ALU.add),
                    reads=['mss'], writes=['mss2'])
                add('pool', lambda e: e.tensor_tensor(out=mrq, in0=mss2, in1=mhalf[:, 0:4], op=ALU.pow), reads=['mss2', 'mhalf'], writes=['mrq'])
                add('dve', lambda e: e.tensor_tensor(out=mkn.rearrange("p (h d) -> p h d", h=4), in0=mks.rearrange("p (h d) -> p h d", h=4),
                                                     in1=mrq.unsqueeze(2).broadcast_to([128, 4, 64]), op=ALU.mult),
                    reads=['mks', 'mrq'], writes=['mkn'])
                pq = PB[3][:].bitcast(BF16)
                for c in range(2):
                    add('pe', lambda e, c=c, pq=pq: e.transpose(out=pq[:, c * 128:(c + 1) * 128], in_=mkn[:, c * 128:(c + 1) * 128], identity=identb[:]),
                        reads=['mkn', 'identb'], writes=[('pb', 3)])
                for h in range(4):
                    c, hf = h // 2, h % 2
                    lo, hi = hf * 64, hf * 64 + 64
                    add('dve', lambda e, mt=mt, pq=pq, h=h, c=c, lo=lo, hi=hi: e.tensor_scalar(out=mkz[lo:hi, h, mt * 128:(mt + 1) * 128], in0=pq[lo:hi, c * 128:(c + 1) * 128],
                                                                                               scalar1=gvec[lo:hi, GC_QK + l * 8 + 7:GC_QK + l * 8 + 8], scalar2=None, op0=ALU.mult),
                        reads=[('pb', 3), 'gvec'], writes=['mkz'])

            if DEBUG_STAGE == 'mem':
                return
            for i in range(NT if DEBUG_STAGE != 'tile1' else 2):
                p = i % 2
                xi = X[:, i, :]
                norm_T(xi, ('x', i), gvec[:, GC_MIX + l * 8:GC_MIX + l * 8 + 8], hTt[p], ('hTt', p), (0, 1), xn_m, junk_m, 'm')
                for half in range(2):
                    for kc in range(8):
                        add('pe', lambda e, kc=kc, half=half, p=p: e.matmul(PB[2 + half][:], lhsT=hTt[p][:, kc, :], rhs=Win[:, kc, half * 512:(half + 1) * 512],
                                                                          start=(kc == 0), stop=(kc == 7)),
                            reads=[('hTt', p), 'Win'], writes=[('pb', 2 + half)])
                for cc in range(6):
                    b = 4 + cc // 4
                    for kc in range(8):
                        add('pe', lambda e, kc=kc, cc=cc, b=b, p=p: e.matmul(PB[b][:, (cc % 4) * 128:(cc % 4 + 1) * 128],
                                                                           lhsT=Win[:, kc, 1024 + cc * 128:1024 + (cc + 1) * 128], rhs=hTt[p][:, kc, :],
                                                                           start=(kc == 0), stop=(kc == 7)),
                            reads=[('hTt', p), 'Win'], writes=[('pb', b)])
                add('act', lambda e: e.activation(out=qk[:, 0:512], in_=PB[2][:], func=AF.Copy), reads=[('pb', 2)], writes=['qk'])
                add('act', lambda e: e.activation(out=qk[:, 512:896], in_=PB[3][:, 0:384], func=AF.Copy), reads=[('pb', 3)], writes=['qk'])
                add('act', lambda e, p=p: e.activation(out=Vaug[p][:, :, 0:64], in_=PB[3][:, 384:512].rearrange("p (h d) -> p h d", h=2), func=AF.Copy),
                    reads=[('pb', 3)], writes=[('Vaug', p)])
                add('dve', lambda e: e.tensor_tensor(out=qsq, in0=qk, in1=qk, op=ALU.mult), reads=['qk'], writes=['qsq'])
                add('dve', lambda e: e.reduce_sum(out=ssq, in_=qsq.rearrange("p (h d) -> p h d", h=14), axis=AX.X), reads=['qsq'], writes=['ssq'])
                add('dve', lambda e: e.tensor_scalar(out=ssq2, in0=ssq, scalar1=1.0 / 64, scalar2=EPS, op0=ALU.mult, op1=ALU.add),
                    reads=['ssq'], writes=['ssq2'])
                add('pool', lambda e: e.tensor_tensor(out=rq, in0=ssq2, in1=mhalf[:, 0:14], op=ALU.pow), reads=['ssq2', 'mhalf'], writes=['rq'])
                add('dve', lambda e: e.tensor_tensor(out=qkn.rearrange("p (h d) -> p h d", h=14), in0=qk.rearrange("p (h d) -> p h d", h=14),
                                                     in1=rq.unsqueeze(2).broadcast_to([128, 14, 64]), op=ALU.mult),
                    reads=['qk', 'rq'], writes=['qkn'])
                pq = PB[6][:].bitcast(BF16)
                for c in range(7):
                    add('pe', lambda e, c=c, pq=pq: e.transpose(out=pq[:, c * 128:(c + 1) * 128], in_=qkn[:, c * 128:(c + 1) * 128], identity=identb[:]),
                        reads=['qkn', 'identb'], writes=[('pb', 6)])
                add('dve', lambda e, p=p, pq=pq: e.tensor_tensor(out=qTt[p], in0=pq[:, 0:896].rearrange("p (c t) -> p c t", c=7),
                                                                 in1=gvec[:, GC_QK + l * 8:GC_QK + l * 8 + 7].unsqueeze(2).broadcast_to([128, 7, 128]), op=ALU.mult),
                    reads=[('pb', 6), 'gvec'], writes=[('qTt', p)])
                for kv in range(2):
                    lo, hi = kv * 64, kv * 64 + 64
                    add('dve', lambda e, p=p, pq=pq, kv=kv, lo=lo, hi=hi: e.tensor_scalar(out=kz[p][kv][lo:hi, :], in0=pq[lo:hi, 512:640],
                                                                                          scalar1=gvec[lo:hi, GC_QK + l * 8 + 4:GC_QK + l * 8 + 5], scalar2=None, op0=ALU.mult),
                        reads=[('pb', 6), 'gvec'], writes=[('kz', p)])
                if DEBUG_LEVEL < 1:
                    continue
                rels = [1] if i == 0 else [0, 1]
                blk = 0
                for kvh in range(2):
                    lo, hi = kvh * 64, kvh * 64 + 64
                    for r in rels:
                        kp = p if r == 1 else 1 - p
                        b = blk % 2
                        add('pe', lambda e, kp=kp, p=p, kvh=kvh, b=b: e.matmul(PB[b][:], lhsT=kz[kp][kvh][:], rhs=qTt[p][:, 0:4, :],
                                                                             start=True, stop=True),
                            reads=[('kz', kp), ('qTt', p)], writes=[('pb', b)])
                        add('act', lambda e, b=b: e.activation(out=es[b], in_=PB[b][:], func=AF.Exp, scale=0.125),
                            reads=[('pb', b)], writes=[('es', b)])
                        add('dve', lambda e, b=b, r=r, kvh=kvh: e.tensor_tensor(out=PT[:, kvh * 2 + r, :].rearrange("p (h q) -> p h q", h=4),
                                                                                in0=es[b].rearrange("p (h q) -> p h q", h=4),
                                                                                in1=ET[:, r, kvh * 4:kvh * 4 + 4, :], op=ALU.mult),
                            reads=[('es', b), 'ET'], writes=[('PT', kvh * 2 + r)])
                        blk += 1
                if DEBUG_LEVEL < 2:
                    continue
                for h in range(8):
                    kvh, g = h // 4, h % 4
                    b = 2 + kvh
                    po = PB[b][:, 0:260].rearrange("p (h d) -> p h d", h=4)
                    for ri, r in enumerate(rels):
                        kp = p if r == 1 else 1 - p
                        lastr = (ri == len(rels) - 1)
                        add('pe', lambda e, po=po, g=g, kvh=kvh, r=r, kp=kp, ri=ri, lastr=lastr: e.matmul(po[:, g, :], lhsT=PT[:, kvh * 2 + r, g * 128:(g + 1) * 128],
                                                                                           rhs=Vaug[kp][:, kvh, :], start=(ri == 0), stop=lastr),
                            reads=[('PT', kvh * 2 + r), ('Vaug', kp)], writes=[('pb', b)])
                if DEBUG_LEVEL < 3:
                    continue
                for mt in range(2):
                    b = 6 + mt
                    for h in range(4):
                        c, hf = h // 2, h % 2
                        lo, hi = hf * 64, hf * 64 + 64
                        add('pe', lambda e, b=b, h=h, c=c, mt=mt, p=p: e.matmul(PB[b][:, h * 128:(h + 1) * 128], lhsT=mkz[:, h, mt * 128:(mt + 1) * 128],
                                                                              rhs=qTt[p][:, 5 + c, :], start=True, stop=True),
                            reads=['mkz', ('qTt', p)], writes=[('pb', b)])
                    add('act', lambda e, b=b, mt=mt: e.activation(out=PmT[:, mt, :], in_=PB[b][:], func=AF.Exp, scale=0.125),
                        reads=[('pb', b)], writes=[('PmT', mt)])
                pom = PB[0][:, 0:260].rearrange("p (h d) -> p h d", h=4)
                for h in range(4):
                    for mt in range(2):
                        add('pe', lambda e, h=h, mt=mt, pom=pom: e.matmul(pom[:, h, :], lhsT=PmT[:, mt, h * 128:(h + 1) * 128], rhs=mvA[:, mt, h, :],
                                                                         start=(mt == 0), stop=(mt == 1)),
                            reads=[('PmT', mt), 'mvA'], writes=[('pb', 0)])
                if DEBUG_LEVEL < 4:
                    continue
                for kvh in range(2):
                    po = PB[2 + kvh][:, 0:260].rearrange("p (h d) -> p h d", h=4)
                    add('dve', lambda e, po=po, kvh=kvh: e.tensor_tensor(out=den[:, kvh * 4:kvh * 4 + 4], in0=po[:, :, 64],
                                                                         in1=esink[:, l * 8 + kvh * 4:l * 8 + kvh * 4 + 4], op=ALU.add),
                        reads=[('pb', 2 + kvh), 'esink'], writes=['den'])
                add('dve', lambda e: e.reciprocal(out=rden, in_=den), reads=['den'], writes=['rden'])
                for kvh in range(2):
                    po = PB[2 + kvh][:, 0:260].rearrange("p (h d) -> p h d", h=4)
                    add('dve', lambda e, po=po, kvh=kvh: e.tensor_tensor(out=ysw[:, kvh * 256:(kvh + 1) * 256].rearrange("p (h d) -> p h d", h=4), in0=po[:, :, 0:64],
                                                                         in1=rden[:, kvh * 4:kvh * 4 + 4].unsqueeze(2).broadcast_to([128, 4, 64]), op=ALU.mult),
                        reads=[('pb', 2 + kvh), 'rden'], writes=['ysw'])
                add('dve', lambda e, pom=pom: e.reciprocal(out=rdm, in_=pom[:, :, 64]), reads=[('pb', 0)], writes=['rdm'])
                add('dve', lambda e, pom=pom: e.tensor_tensor(out=ym.rearrange("p (h d) -> p h d", h=4), in0=pom[:, :, 0:64],
                                                              in1=rdm.unsqueeze(2).broadcast_to([128, 4, 64]), op=ALU.mult),
                    reads=[('pb', 0), 'rdm'], writes=['ym'])
                add('act', lambda e: e.activation(out=junk_m[:, 0:512], in_=ysw, func=AF.Square, accum_out=oss[:, 0:1]),
                    reads=['ysw'], writes=[('m', 'junk'), 'oss'])
                add('act', lambda e: e.activation(out=junk_m[:, 512:768], in_=ym, func=AF.Square, accum_out=oss[:, 1:2]),
                    reads=['ym'], writes=[('m', 'junk'), 'oss'])
                add('dve', lambda e: e.tensor_tensor(out=oss2, in0=oss, in1=oscale, op=ALU.mult), reads=['oss', 'oscale'], writes=['oss2'])
                add('dve', lambda e: e.tensor_scalar(out=oss2, in0=oss2, scalar1=EPS, scalar2=None, op0=ALU.add), reads=['oss2'], writes=['oss2'])
                add('pool', lambda e: e.tensor_tensor(out=orin, in0=oss2, in1=mhalf[:, 0:2], op=ALU.pow), reads=['oss2', 'mhalf'], writes=['orin'])
                add('act', lambda e: e.activation(out=ynb[:, 0:512], in_=ysw, func=AF.Copy, scale=orin[:, 0:1]), reads=['ysw', 'orin'], writes=['ynb'])
                add('act', lambda e: e.activation(out=ynb[:, 512:768], in_=ym, func=AF.Copy, scale=orin[:, 1:2]), reads=['ym', 'orin'], writes=['ynb'])
                pyt = PB[1][:].bitcast(BF16)
                for c in range(6):
                    add('pe', lambda e, c=c, pyt=pyt: e.transpose(out=pyt[:, c * 128:(c + 1) * 128], in_=ynb[:, c * 128:(c + 1) * 128], identity=identb[:]),
                        reads=['ynb', 'identb'], writes=[('pb', 1)])
                go = GC_OUT + l * 8
                add('dve', lambda e, pyt=pyt, go=go: e.tensor_tensor(out=yT[:, 0:4, :], in0=pyt[:, 0:512].rearrange("p (c t) -> p c t", c=4),
                                                                     in1=gvec[:, go:go + 4].unsqueeze(2).broadcast_to([128, 4, 128]), op=ALU.mult),
                    reads=[('pb', 1), 'gvec'], writes=['yT'])
                add('dve', lambda e, pyt=pyt, go=go: e.tensor_tensor(out=yT[:, 6:8, :], in0=pyt[:, 512:768].rearrange("p (c t) -> p c t", c=2),
                                                                     in1=gvec[:, go + 6:go + 8].unsqueeze(2).broadcast_to([128, 2, 128]), op=ALU.mult),
                    reads=[('pb', 1), 'gvec'], writes=['yT'])
                if DEBUG_LEVEL < 5:
                    continue
                pcb = PB[4][:, 0:256].rearrange("p (c t) -> p c t", c=2)
                pcc = PB[4][:, 256:512].rearrange("p (c t) -> p c t", c=2)
                pcu = PB[5][:, 0:256].rearrange("p (c t) -> p c t", c=2)
                z = zt[p]
                zn = zt[1 - p]
                add('act', lambda e, pcc=pcc: e.activation(out=ccs, in_=pcc, func=AF.Copy), reads=[('pb', 4)], writes=['ccs'])
                add('dve', lambda e, z=z, pcu=pcu: e.tensor_tensor(out=z[:, :, 2:130], in0=ccs, in1=pcu, op=ALU.mult),
                    reads=['ccs', ('pb', 5)], writes=[('zt', p)])
                add('pool', lambda e, z=z, zn=zn: e.tensor_copy(out=zn[:, :, 0:2], in_=z[:, :, 128:130]), reads=[('zt', p)], writes=[('zt', 1 - p)])
                cw = GC_CONV + l * 6
                for c in range(2):
                    add('dve', lambda e, z=z, c=c, cw=cw: e.tensor_scalar(out=cacc[:, c, :], in0=z[:, c, 0:128], scalar1=gvec[:, cw + c:cw + c + 1], scalar2=None, op0=ALU.mult),
                        reads=[('zt', p), 'gvec'], writes=['cacc'])
                    for j in (1, 2):
                        add('dve', lambda e, z=z, c=c, j=j, cw=cw: e.scalar_tensor_tensor(out=cacc[:, c, :], in0=z[:, c, j:j + 128], scalar=gvec[:, cw + 2 * j + c:cw + 2 * j + c + 1],
                                                                                        in1=cacc[:, c, :], op0=ALU.mult, op1=ALU.add),
                            reads=[('zt', p), 'gvec', 'cacc'], writes=['cacc'])
                add('dve', lambda e, pcb=pcb: e.tensor_tensor(out=ycv, in0=cacc, in1=pcb, op=ALU.mult), reads=['cacc', ('pb', 4)], writes=['ycv'])
                add('dve', lambda e: e.tensor_tensor(out=csq, in0=ycv, in1=ycv, op=ALU.mult), reads=['ycv'], writes=['csq'])
                for c in range(2):
                    add('pe', lambda e, c=c: e.matmul(PB[5][:, 256:384], lhsT=ones[:], rhs=csq[:, c, :], start=(c == 0), stop=(c == 1)),
                        reads=['ones', 'csq'], writes=[('pb', 5)])
                add('dve', lambda e: e.tensor_scalar(out=rcv, in0=PB[5][:, 256:384], scalar1=1.0 / 256, scalar2=EPS, op0=ALU.mult, op1=ALU.add),
                    reads=[('pb', 5)], writes=['rcv'])
                add('pool', lambda e: e.tensor_tensor(out=rcv, in0=rcv, in1=mhalf[:], op=ALU.pow), reads=['rcv', 'mhalf'], writes=['rcv'])
                for c in range(2):
                    add('dve', lambda e, c=c, go=go: e.scalar_tensor_tensor(out=yT[:, 4 + c, :], in0=ycv[:, c, :], scalar=gvec[:, go + 4 + c:go + 5 + c], in1=rcv,
                                                                            op0=ALU.mult, op1=ALU.mult),
                        reads=['ycv', 'gvec', 'rcv'], writes=['yT'])
                if DEBUG_LEVEL < 6:
                    continue
                for half in range(2):
                    b = 6 + half
                    for kc in range(8):
                        add('pe', lambda e, kc=kc, half=half, b=b: e.matmul(PB[b][:], lhsT=yT[:, kc, :], rhs=Wout[:, kc, half * 512:(half + 1) * 512],
                                                                          start=(kc == 0), stop=(kc == 7)),
                            reads=['yT', 'Wout'], writes=[('pb', b)])
                    add('dve', lambda e, half=half, b=b, i=i: e.tensor_tensor(out=X[:, i, half * 512:(half + 1) * 512], in0=PB[b][:], in1=X[:, i, half * 512:(half + 1) * 512], op=ALU.add),
                        reads=[('pb', b), ('x', i)], writes=[('x', i)])

        gcount = [0]

        def ffn(s, l):
            moe = (l % 2 == 1)
            li = l // 2
            for i in range(NT):
                norm_T(X[:, i, :], ('x', i), gvec[:, GC_FFN + l * 8:GC_FFN + l * 8 + 8], hT[:, :, i * 128:(i + 1) * 128], 'hT', (0, 1), xn_f, junk_f, 'f',
                       f32dst=(h2f if moe else None), f32res='h2f')
                if moe:
                    for kc in range(8):
                        add('pe', lambda e, kc=kc: e.matmul(PB[2][:, 0:8], lhsT=h2f[:, kc, :], rhs=wr[:, li, kc, :], start=(kc == 0), stop=(kc == 7)),
                            reads=['h2f', 'wr'], writes=[('pb', 2)])
                    add('dve', lambda e, i=i: e.tensor_tensor(out=L[:, i, :], in0=PB[2][:, 0:8], in1=brow[:, BR_BR + li * 8:BR_BR + li * 8 + 8], op=ALU.add),
                        reads=[('pb', 2), 'brow'], writes=['L'])
            if moe:
                bc = lambda a: a.unsqueeze(2).broadcast_to([128, NT, 8])
                add('dve', lambda e: e.reduce_max(out=m1, in_=L, axis=AX.X), reads=['L'], writes=['m1'])
                add('dve', lambda e: e.tensor_tensor(out=eq1, in0=L, in1=bc(m1), op=ALU.is_equal), reads=['L', 'm1'], writes=['eq1'])
                add('dve', lambda e: e.scalar_tensor_tensor(out=L2.rearrange("p a b -> p (a b)"), in0=eq1.rearrange("p a b -> p (a b)"), scalar=-1e30,
                                                            in1=L.rearrange("p a b -> p (a b)"), op0=ALU.mult, op1=ALU.add),
                    reads=['eq1', 'L'], writes=['L2'])
                add('dve', lambda e: e.reduce_max(out=m2, in_=L2, axis=AX.X), reads=['L2'], writes=['m2'])
                add('dve', lambda e: e.tensor_tensor(out=eq2, in0=L2, in1=bc(m2), op=ALU.is_equal), reads=['L2', 'm2'], writes=['eq2'])
                add('dve', lambda e: e.tensor_tensor(out=dd, in0=m2, in1=m1, op=ALU.subtract), reads=['m1', 'm2'], writes=['dd'])
                add('act', lambda e: e.activation(out=ee, in_=dd, func=AF.Exp), reads=['dd'], writes=['ee'])
                add('dve', lambda e: e.tensor_scalar(out=g1, in0=ee, scalar1=1.0, scalar2=None, op0=ALU.add), reads=['ee'], writes=['g1'])
                add('dve', lambda e: e.reciprocal(out=g1, in_=g1), reads=['g1'], writes=['g1'])
                add('dve', lambda e: e.tensor_tensor(out=g2, in0=ee, in1=g1, op=ALU.mult), reads=['ee', 'g1'], writes=['g2'])
                add('dve', lambda e: e.tensor_tensor(out=eq1, in0=eq1, in1=bc(g1), op=ALU.mult), reads=['eq1', 'g1'], writes=['eq1'])
                add('dve', lambda e: e.tensor_tensor(out=eq2, in0=eq2, in1=bc(g2), op=ALU.mult), reads=['eq2', 'g2'], writes=['eq2'])
                add('dve', lambda e: e.tensor_tensor(out=comb, in0=eq1, in1=eq2, op=ALU.add), reads=['eq1', 'eq2'], writes=['comb'])

            experts = list(range(N_EXP)) if moe else [None]
            groups = [(ex, g) for ex in experts for g in range(NG)]

            def issue_dma(ex, g):
                gi = gcount[0]
                gcount[0] += 1
                b = gi % NWB
                Wg_, Wu_, Wd_ = Wb[b]
                if ex is None:
                    sg_, su_, sd_ = wgd_d[li], wud_d[li], wdd_d[li]
                else:
                    sg_, su_, sd_ = wgm_d[li, ex], wum_d[li, ex], wdm_d[li, ex]
                add('pool', lambda e: e.dma_start(out=Wg_, in_=sg_[:, g * GW:(g + 1) * GW].rearrange("(kc p) n -> p kc n", p=128)),
                    writes=[('Wg', b)], dma=True, dsem=('Wg', b))
                add('pool', lambda e: e.dma_start(out=Wu_, in_=su_[:, g * GW:(g + 1) * GW].rearrange("(kc p) n -> p kc n", p=128)),
                    writes=[('Wu', b)], dma=True, dsem=('Wu', b))
                add('pool', lambda e: e.dma_start(out=Wd_, in_=sd_[g * GW:(g + 1) * GW, :].rearrange("(fc p) n -> p fc n", p=128)),
                    writes=[('Wd', b)], dma=True, dsem=('Wd', b))
                return b

            bufs = {}
            PRE = NWB - 1
            for k in range(min(PRE, len(groups))):
                bufs[k] = issue_dma(*groups[k])
            pending = [None]
            step = [0]
            dcount = [0]

            def do_down(ex, b, tg, ap):
                Wd_ = Wb[b][2]
                for tt in range(4):
                    tile = tg * 4 + tt
                    for half in range(2):
                        db = 4 + dcount[0] % 4
                        dcount[0] += 1
                        for fcl in range(GW // 128):
                            add('pe', lambda e, db=db, fcl=fcl, tt=tt, half=half: e.matmul(PB[db][:], lhsT=AT[ap][:, fcl, tt * 128:(tt + 1) * 128],
                                                                                         rhs=Wd_[:, fcl, half * 512:(half + 1) * 512],
                                                                                         start=(fcl == 0), stop=(fcl == GW // 128 - 1)),
                                reads=[('AT', ap), ('Wd', b)], writes=[('pb', db)])
                        xs = X[:, tile, half * 512:(half + 1) * 512]
                        if ex is None:
                            add('dve', lambda e, db=db, xs=xs: e.tensor_tensor(out=xs, in0=PB[db][:], in1=xs, op=ALU.add),
                                reads=[('pb', db), ('x', tile)], writes=[('x', tile)])
                        else:
                            add('dve', lambda e, db=db, xs=xs, tile=tile: e.scalar_tensor_tensor(out=xs, in0=PB[db][:], scalar=comb[:, tile, ex:ex + 1], in1=xs,
                                                                                               op0=ALU.mult, op1=ALU.add),
                                reads=[('pb', db), ('x', tile), 'comb'], writes=[('x', tile)])

            for k, (ex, g) in enumerate(groups):
                b = bufs[k]
                Wg_, Wu_, _ = Wb[b]
                for tg in range(4):
                    ap = step[0] % 2
                    step[0] += 1
                    for fcl in range(GW // 128):
                        gb = fcl % 2
                        ub = 2 + fcl % 2
                        for kc in range(8):
                            add('pe', lambda e, kc=kc, fcl=fcl, gb=gb, tg=tg, Wg_=Wg_: e.matmul(PB[gb][:], lhsT=Wg_[:, kc, fcl * 128:(fcl + 1) * 128], rhs=hT[:, kc, tg * 512:(tg + 1) * 512],
                                                                                     start=(kc == 0), stop=(kc == 7)),
                                reads=[('Wg', b), 'hT'], writes=[('pb', gb)])
                        for kc in range(8):
                            add('pe', lambda e, kc=kc, fcl=fcl, ub=ub, tg=tg, Wu_=Wu_: e.matmul(PB[ub][:], lhsT=Wu_[:, kc, fcl * 128:(fcl + 1) * 128], rhs=hT[:, kc, tg * 512:(tg + 1) * 512],
                                                                                     start=(kc == 0), stop=(kc == 7)),
                                reads=[('Wu', b), 'hT'], writes=[('pb', ub)])
                        add('act', lambda e, gb=gb: e.activation(out=sg[gb], in_=PB[gb][:], func=AF.Silu), reads=[('pb', gb)], writes=[('sg', gb)])
                        add('dve', lambda e, gb=gb, ub=ub, fcl=fcl, ap=ap: e.tensor_tensor(out=AT[ap][:, fcl, :], in0=sg[gb], in1=PB[ub][:], op=ALU.mult),
                            reads=[('sg', gb), ('pb', ub)], writes=[('AT', ap)])
                    if pending[0] is not None:
                        do_down(*pending[0])
                    pending[0] = (ex, b, tg, ap)
                    if tg == 0 and k + PRE < len(groups):
                        bufs[k + PRE] = issue_dma(*groups[k + PRE])
            do_down(*pending[0])

        for s in range(n_seq):
            for q4 in range(4):
                add('sp', lambda e, q4=q4, s=s: e.dma_start(out=X[:, q4 * 4:(q4 + 1) * 4, :], in_=x_d[s, q4 * 512:(q4 + 1) * 512, :].rearrange("(t p) d -> p t d", p=128)),
                    writes=[('x', q4 * 4 + t) for t in range(4)], dma=True, dsem=('xin', q4))
            for l in layers:
                S.barrier()
                mixer(s, l)
                S.barrier()
                if DEBUG_STAGE in ('mem', 'tile1', 'mixer'):
                    continue
                ffn(s, l)
            for q4 in range(4):
                add('sp', lambda e, q4=q4, s=s: e.dma_start(out=out_d[s, q4 * 512:(q4 + 1) * 512, :].rearrange("(t p) d -> p t d", p=128), in_=X[:, q4 * 4:(q4 + 1) * 4, :]),
                    reads=[('x', q4 * 4 + t) for t in range(4)], dma=True, dsem=('xout', q4))
        S.emit_all()
    return nc


def _alibi_table():
    j = np.arange(128)[:, None, None, None].astype(np.float64)
    r = np.arange(2)[None, :, None, None]
    h = np.arange(8)[None, None, :, None]
    i = np.arange(128)[None, None, None, :].astype(np.float64)
    kpos = j + (r - 1) * 128
    kc = np.floor(kpos / 64)
    qc = np.floor(i / 64)
    valid = (kc >= qc - 2) & (kc <= qc)
    slope = 2.0 ** (-(h + 1.0))
    t = np.where(valid, np.exp(-slope * np.abs(i - kpos)), 0.0)
    return np.ascontiguousarray(t.reshape(128, 2 * 8 * 128)).astype(np.float32)


def _prep_small(inp):
    f = lambda a: np.asarray(a, dtype=np.float32)
    gvec = np.zeros((128, GC_N), np.float32)
    g_out = np.concatenate([f(inp['g_out_swa']), f(inp['g_out_conv']), f(inp['g_out_mem'])], axis=1)
    for l in range(DEPTH):
        gvec[:, GC_MIX + l * 8:GC_MIX + l * 8 + 8] = f(inp['g_mix'])[l].reshape(8, 128).T
        gvec[:, GC_FFN + l * 8:GC_FFN + l * 8 + 8] = f(inp['g_ffn'])[l].reshape(8, 128).T
        gvec[:, GC_MEM + l * 8:GC_MEM + l * 8 + 8] = f(inp['g_mem'])[l].reshape(8, 128).T
        gvec[:, GC_OUT + l * 8:GC_OUT + l * 8 + 8] = g_out[l].reshape(8, 128).T
        gq = np.tile(f(inp['g_q_swa'])[l], 2)
        gk = np.tile(f(inp['g_k_swa'])[l], 2)
        gqm = np.tile(f(inp['g_q_mem'])[l], 2)
        gkm = np.tile(f(inp['g_k_mem'])[l], 2)
        for c in range(4):
            gvec[:, GC_QK + l * 8 + c] = gq
        gvec[:, GC_QK + l * 8 + 4] = gk
        gvec[:, GC_QK + l * 8 + 5] = gqm
        gvec[:, GC_QK + l * 8 + 6] = gqm
        gvec[:, GC_QK + l * 8 + 7] = gkm
        cw = f(inp['conv_w'])[l]
        for j in range(3):
            for c in range(2):
                gvec[:, GC_CONV + l * 6 + 2 * j + c] = cw[j, c * 128:(c + 1) * 128]
    brow = np.zeros((128, BR_N), np.float32)
    brow[:, BR_SINK:BR_SINK + 32] = f(inp['sinks']).reshape(1, 32)
    brow[:, BR_BR:BR_BR + 16] = f(inp['b_router']).reshape(1, 16)
    wrr = f(inp['w_router']).reshape(2, 8, 128, 8).transpose(2, 0, 1, 3).reshape(128, 128)
    return gvec, brow, np.ascontiguousarray(wrr)


def _perm_w_in(w_in):
    heads = [0, 4, 1, 5, 2, 6, 3, 7]
    cols = []
    for h in heads:
        cols += list(range(h * 64, (h + 1) * 64))
    cols += list(range(512, 640))
    cols += list(range(1536, 1792))
    cols += list(range(640, 768))
    cols += list(range(768, 1536))
    return np.ascontiguousarray(np.asarray(w_in, dtype=np.float32)[:, :, cols])


def kernel(**inp):
    n_cores = 8
    x = np.asarray(inp['x'], dtype=np.float32)
    mem = np.asarray(inp['mem'], dtype=np.float32)
    B = x.shape[0]
    n_seq = B // n_cores
    gvec, brow, wrr = _prep_small(inp)
    shared = {
        "w_in": _perm_w_in(inp['w_in']),
        "w_mem_kv": np.asarray(inp['w_mem_kv'], dtype=np.float32),
        "w_out": np.asarray(inp['w_out'], dtype=np.float32),
        "w_gate_dense": np.asarray(inp['w_gate_dense'], dtype=np.float32),
        "w_up_dense": np.asarray(inp['w_up_dense'], dtype=np.float32),
        "w_down_dense": np.asarray(inp['w_down_dense'], dtype=np.float32),
        "w_gate_moe": np.asarray(inp['w_gate_moe'], dtype=np.float32),
        "w_up_moe": np.asarray(inp['w_up_moe'], dtype=np.float32),
        "w_down_moe": np.asarray(inp['w_down_moe'], dtype=np.float32),
        "gvec": gvec, "brow": brow, "wr": wrr, "et": _alibi_table(),
        "ident": np.eye(128, dtype=np.float32),
    }
    nc = build_program(layers=tuple(range(DEPTH)), n_seq=n_seq)
    in_maps = []
    for c in range(n_cores):
        m = dict(shared)
        m["x"] = np.ascontiguousarray(x[c * n_seq:(c + 1) * n_seq])
        m["mem"] = np.ascontiguousarray(mem[c * n_seq:(c + 1) * n_seq])
        in_maps.append(m)
    res = run_bass_kernel_spmd(nc, in_maps, core_ids=list(range(n_cores)))
    out = np.concatenate([np.asarray(r["out"]) for r in res.results], axis=0)
    return out.astype(np.float32)
```

```python
import contextlib
import numpy as np
import concourse.bass as bass
import concourse.mybir as mybir
from concourse.bass_utils import run_bass_kernel_spmd

F32 = mybir.dt.float32
BF16 = mybir.dt.bfloat16
AF = mybir.ActivationFunctionType
ALU = mybir.AluOpType
AX = mybir.AxisListType

D_MODEL = 1024
SEQ = 2048
NT = SEQ // 128
DEPTH = 4
D_FF = 3584
N_EXP = 8
MEM_LEN = 256
EPS = 1e-6
GW = 512
NG = D_FF // GW
NWB = 3

ENGS = ['pe', 'act', 'dve', 'pool', 'sp']
DEBUG_STAGE = None
DEBUG_LEVEL = 99


class Op:
    __slots__ = ('eng', 'emit', 'deps', 'raw', 'is_dma', 'dsem', 'dval', 'sig', 'cnt', 'idx')


class Sched:
    def __init__(self, nc):
        self.nc = nc
        self.ops = []
        self.per_eng = {e: [] for e in ENGS}
        self.last_w = {}
        self.readers = {}
        self.dma_val = {}
        self.pending_bar = {}

    def add(self, eng, emit, reads=(), writes=(), dma=False, dsem=None):
        op = Op()
        op.eng = eng; op.emit = emit; op.is_dma = dma; op.idx = len(self.ops)
        op.sig = False; op.cnt = 0; op.dsem = None; op.dval = 0
        deps = set()
        for r in reads:
            w = self.last_w.get(r)
            if w is not None:
                deps.add(w)
        op.raw = set(deps)
        for r in writes:
            w = self.last_w.get(r)
            if w is not None:
                deps.add(w)
            for rd in self.readers.get(r, ()):
                deps.add(rd)
        pb = self.pending_bar.pop(eng, None)
        if pb:
            deps |= pb
        for r in reads:
            self.readers.setdefault(r, []).append(op.idx)
        for r in writes:
            self.last_w[r] = op.idx
            self.readers[r] = []
        if dma:
            v = self.dma_val.get(dsem, 0) + 16
            self.dma_val[dsem] = v
            op.dsem = dsem; op.dval = v
        op.deps = deps
        self.ops.append(op)
        self.per_eng[eng].append(op)
        return op

    def barrier(self):
        last = set()
        for e in ENGS:
            if self.per_eng[e]:
                last.add(self.per_eng[e][-1].idx)
        for e in ENGS:
            self.pending_bar[e] = set(last)

    def finalize(self):
        ops = self.ops
        for op in ops:
            for d in op.deps:
                dop = ops[d]
                if dop.is_dma:
                    continue
                if dop.eng == 'pe' and op.eng == 'pe' and not op.is_dma:
                    continue
                if dop.eng == op.eng and not op.is_dma and d not in op.raw:
                    continue
                dop.sig = True
        for e in ENGS:
            c = 0
            for op in self.per_eng[e]:
                if op.sig and not op.is_dma:
                    c += 1
                op.cnt = c

    def emit_all(self, final_waits_eng='sp'):
        nc = self.nc
        self.finalize()
        with contextlib.ExitStack() as st:
            esem = {e: st.enter_context(nc.semaphore('s_' + e)) for e in ENGS}
            dsem = {k: st.enter_context(nc.semaphore('d_%d' % i)) for i, k in enumerate(self.dma_val)}
            block = st.enter_context(nc.Block())
            ops = self.ops

            def run(e, eng):
                waited = {}
                for op in self.per_eng[e]:
                    for d in sorted(op.deps):
                        dop = ops[d]
                        if dop.is_dma:
                            key = ('d', dop.dsem); sem = dsem[dop.dsem]; val = dop.dval
                        else:
                            if dop.eng == 'pe' and e == 'pe' and not op.is_dma:
                                continue
                            if dop.eng == e and not op.is_dma and d not in op.raw:
                                continue
                            key = ('e', dop.eng); sem = esem[dop.eng]; val = dop.cnt
                        if waited.get(key, 0) >= val:
                            continue
                        waited[key] = val
                        eng.wait_ge(sem, val)
                    ins = op.emit(eng)
                    if op.is_dma:
                        ins.then_inc(dsem[op.dsem], 16)
                    elif op.sig:
                        ins.then_inc(esem[e], 1)
                if e == final_waits_eng:
                    for k, v in self.dma_val.items():
                        if waited.get(('d', k), 0) < v:
                            eng.wait_ge(dsem[k], v)

            @block.tensor
            def _(eng):
                run('pe', eng)

            @block.scalar
            def _(eng):
                run('act', eng)

            @block.vector
            def _(eng):
                run('dve', eng)

            @block.gpsimd
            def _(eng):
                run('pool', eng)

            @block.sync
            def _(eng):
                run('sp', eng)


GC_MIX, GC_FFN, GC_MEM, GC_OUT, GC_QK, GC_CONV = 0, 32, 64, 96, 128, 160
GC_N = 184
BR_SINK, BR_BR = 0, 32
BR_N = 48


def build_program(layers=(0, 1, 2, 3), n_seq=2):
    nc = bass.Bass("TRN2", target_bir_lowering=False)

    def din(name, shape):
        return nc.dram_tensor(name, list(shape), F32, kind="ExternalInput").ap()

    x_d = din("x", [n_seq, SEQ, D_MODEL])
    mem_d = din("mem", [n_seq, MEM_LEN, D_MODEL])
    w_in_d = din("w_in", [DEPTH, D_MODEL, 1792])
    w_mem_d = din("w_mem_kv", [DEPTH, D_MODEL, 512])
    w_out_d = din("w_out", [DEPTH, D_MODEL, D_MODEL])
    wgd_d = din("w_gate_dense", [2, D_MODEL, D_FF])
    wud_d = din("w_up_dense", [2, D_MODEL, D_FF])
    wdd_d = din("w_down_dense", [2, D_FF, D_MODEL])
    wgm_d = din("w_gate_moe", [2, N_EXP, D_MODEL, D_FF])
    wum_d = din("w_up_moe", [2, N_EXP, D_MODEL, D_FF])
    wdm_d = din("w_down_moe", [2, N_EXP, D_FF, D_MODEL])
    gvec_d = din("gvec", [128, GC_N])
    brow_d = din("brow", [128, BR_N])
    wr_d = din("wr", [128, 128])
    et_d = din("et", [128, 2 * 8 * 128])
    ident_d = din("ident", [128, 128])
    out_d = nc.dram_tensor("out", [n_seq, SEQ, D_MODEL], F32, kind="ExternalOutput").ap()

    S = Sched(nc)
    with contextlib.ExitStack() as st:
        def sb(name, shape, dt):
            return st.enter_context(nc.sbuf_tensor(name, shape, dt))

        X = sb("X", [128, NT, D_MODEL], F32)
        gvec = sb("gvec_s", [128, GC_N], F32)
        brow = sb("brow_s", [128, BR_N], F32)
        esink = sb("esink", [128, 32], F32)
        wr = sb("wr_s", [128, 2, 8, 8], F32)
        ET = sb("ET", [128, 2, 8, 128], F32)
        ident = sb("ident_s", [128, 128], F32)
        identb = sb("identb", [128, 128], BF16)
        ones = sb("ones_s", [128, 2], F32)
        mhalf = sb("mhalf", [128, 16], F32)
        small = sb("small", [128, 256], F32)
        UN = 66 * 1024
        U = sb("U", [128, UN], BF16)
        PB = [st.enter_context(nc.psum_tensor("pb%d" % k, [128, 512], F32)) for k in range(8)]

        class Carver:
            def __init__(self):
                self.off = 0

            def take(self, shape, dt):
                n = int(np.prod(shape[1:]))
                nb = n * (4 if dt == F32 else 2)
                nb = (nb + 63) // 64 * 64
                a = U[:, self.off // 2:(self.off + nb) // 2]
                self.off += nb
                assert self.off <= UN * 2, (self.off, UN * 2)
                if dt == F32:
                    a = a.bitcast(F32)
                a = a[:, 0:n]
                if len(shape) == 3:
                    a = a.rearrange("p (a b) -> p a b", a=shape[1])
                elif len(shape) == 4:
                    a = a.rearrange("p (a b c) -> p a b c", a=shape[1], b=shape[2])
                return a

        cm = Carver()
        Win = cm.take([128, 8, 1792], BF16)
        Wout = cm.take([128, 8, 1024], BF16)
        Wmem = cm.take([128, 8, 512], BF16)
        memx = cm.take([128, 2, 1024], F32)
        hmT = cm.take([128, 8, 256], BF16)
        mkT = cm.take([128, 2, 256], BF16)
        mvA = cm.take([128, 2, 4, 65], BF16)
        mks = cm.take([128, 256], F32)
        mksq = cm.take([128, 256], F32)
        mkn = cm.take([128, 256], BF16)
        xn_m = cm.take([128, 1024], F32)
        junk_m = cm.take([128, 1024], BF16)
        hTt = [cm.take([128, 8, 128], BF16) for _ in range(2)]
        qk = cm.take([128, 896], F32)
        qsq = cm.take([128, 896], F32)
        qkn = cm.take([128, 896], BF16)
        qTt = [cm.take([128, 7, 128], BF16) for _ in range(2)]
        Vaug = [cm.take([128, 2, 65], BF16) for _ in range(3)]
        kz = [[cm.take([128, 128], BF16) for _ in range(2)] for _ in range(3)]
        ycT = [cm.take([128, 2, 128], BF16) for _ in range(2)]
        junk2 = cm.take([128, 768], BF16)
        mkz = cm.take([128, 4, 256], BF16)
        es = [cm.take([128, 512], F32) for _ in range(2)]
        PT = cm.take([128, 4, 512], BF16)
        PmT = cm.take([128, 2, 512], BF16)
        ysw = cm.take([128, 512], F32)
        ym = cm.take([128, 256], F32)
        ynb = cm.take([128, 768], BF16)
        yT = cm.take([128, 8, 128], BF16)
        ccs = cm.take([128, 2, 128], F32)
        zt = [cm.take([128, 2, 130], F32) for _ in range(2)]
        cacc = cm.take([128, 2, 128], F32)
        ycv = cm.take([128, 2, 128], F32)
        csq = cm.take([128, 2, 128], F32)
        rcv = cm.take([128, 128], F32)
        mixer_bytes = cm.off
        cf = Carver()
        hT = cf.take([128, 8, SEQ], BF16)
        Wb = [(cf.take([128, 8, GW], BF16), cf.take([128, 8, GW], BF16), cf.take([128, GW // 128, 1024], BF16))
              for _ in range(NWB)]
        AT = [cf.take([128, GW // 128, 512], BF16) for _ in range(2)]
        sg = [cf.take([128, 512], BF16) for _ in range(2)]
        xn_f = cf.take([128, 1024], F32)
        h2f = [cf.take([128, 8, 128], F32) for _ in range(2)]
        L = cf.take([128, NT, 8], F32)
        L2 = cf.take([128, NT, 8], F32)
        eq1 = cf.take([128, NT, 8], F32)
        eq2 = cf.take([128, NT, 8], F32)
        comb = cf.take([128, NT, 8], F32)
        ffn_bytes = cf.off
        if DEBUG_STAGE is not None:
            print('mixer_bytes', mixer_bytes, 'ffn_bytes', ffn_bytes, 'U bytes', UN * 2)

        sm_off = [0]

        def smalloc(n):
            a = small[:, sm_off[0]:sm_off[0] + n]
            sm_off[0] += n
            assert sm_off[0] <= 256
            return a

        ss_ = [smalloc(1) for _ in range(2)]
        ss2_ = [smalloc(1) for _ in range(2)]
        rinv_ = [smalloc(1) for _ in range(2)]
        ssq = smalloc(14); ssq2 = smalloc(14); rq = smalloc(14)
        mss = smalloc(4); mss2 = smalloc(4); mrq = smalloc(4)
        den = smalloc(8); rden = smalloc(8)
        denm = smalloc(4); rdm = smalloc(4)
        oss = smalloc(2); oss2 = smalloc(2); oss3 = smalloc(2); orin = smalloc(2)
        oscale = smalloc(2)
        rcs = [smalloc(1) for _ in range(2)]
        rcinv = [smalloc(1) for _ in range(2)]
        m1 = smalloc(16); m2 = smalloc(16); dd = smalloc(16); ee = smalloc(16); g1 = smalloc(16); g2 = smalloc(16)

        add = S.add
        nstat = [0]

        add('sp', lambda e: e.dma_start(out=gvec[:], in_=gvec_d), writes=['gvec'], dma=True, dsem='gvec')
        add('sp', lambda e: e.dma_start(out=brow[:], in_=brow_d), writes=['brow'], dma=True, dsem='brow')
        add('sp', lambda e: e.dma_start(out=wr[:], in_=wr_d.rearrange("p (m k e) -> p m k e", m=2, k=8)),
            writes=['wr'], dma=True, dsem='wr')
        add('sp', lambda e: e.dma_start(out=ET[:], in_=et_d.rearrange("p (r h q) -> p r h q", r=2, h=8)),
            writes=['ET'], dma=True, dsem='ET')
        add('sp', lambda e: e.dma_start(out=ident[:], in_=ident_d), writes=['ident'], dma=True, dsem='ident')
        add('pool', lambda e: e.memset(mhalf[:], -0.5), writes=['mhalf'])
        add('pool', lambda e: e.memset(ones[:], 1.0), writes=['ones'])
        add('pool', lambda e: e.memset(oscale[:, 0:1], 1.0 / 512), writes=['oscale'])
        add('pool', lambda e: e.memset(oscale[:, 1:2], 1.0 / 256), writes=['oscale'])
        add('act', lambda e: e.activation(out=identb[:], in_=ident[:], func=AF.Copy), reads=['ident'], writes=['identb'])
        add('act', lambda e: e.activation(out=esink[:], in_=brow[:, BR_SINK:BR_SINK + 32], func=AF.Exp),
            reads=['brow'], writes=['esink'])

        def round_robin(gens):
            gens = list(gens)
            while gens:
                for g in list(gens):
                    try:
                        next(g)
                    except StopIteration:
                        gens.remove(g)

        def norm_T_gen(src, src_res, gcol, dst, dst_res, banks, xn, xn_res, junk, junk_res, f32dst=None, f32res=None):
            k = nstat[0] % 2
            nstat[0] += 1
            ss, ss2, rinv = ss_[k], ss2_[k], rinv_[k]
            add('act', lambda e: e.activation(out=junk, in_=src, func=AF.Square, accum_out=ss),
                reads=[src_res], writes=[junk_res, ('ss', k)])
            yield
            add('dve', lambda e: e.tensor_scalar(out=ss2, in0=ss, scalar1=1.0 / D_MODEL, scalar2=EPS,
                                                 op0=ALU.mult, op1=ALU.add), reads=[('ss', k)], writes=[('ss2', k)])
            yield
            add('pool', lambda e: e.tensor_tensor(out=rinv, in0=ss2, in1=mhalf[:, 0:1], op=ALU.pow),
                reads=[('ss2', k), 'mhalf'], writes=[('rinv', k)])
            yield
            add('act', lambda e: e.activation(out=xn, in_=src, func=AF.Copy, scale=rinv),
                reads=[src_res, ('rinv', k)], writes=[xn_res])
            yield
            for kc in range(8):
                b = banks[kc // 4]
                add('pe', lambda e, kc=kc, b=b: e.transpose(out=PB[b][:, (kc % 4) * 128:(kc % 4 + 1) * 128],
                                                            in_=xn[:, kc * 128:(kc + 1) * 128], identity=ident[:]),
                    reads=[xn_res, 'ident'], writes=[('pb', b)])
            yield
            for j in range(2):
                b = banks[j]
                pin = PB[b][:].rearrange("p (a b) -> p a b", a=4)
                gin = gcol[:, 4 * j:4 * j + 4].unsqueeze(2).broadcast_to([128, 4, 128])
                if f32dst is not None:
                    add('dve', lambda e, j=j, pin=pin, gin=gin: e.tensor_tensor(out=f32dst[:, 4 * j:4 * j + 4, :], in0=pin, in1=gin, op=ALU.mult),
                        reads=[('pb', b), 'gvec'], writes=[f32res])
                add('dve', lambda e, j=j, pin=pin, gin=gin: e.tensor_tensor(out=dst[:, 4 * j:4 * j + 4, :], in0=pin, in1=gin, op=ALU.mult),
                    reads=[('pb', b), 'gvec'], writes=[dst_res])
            yield

        def norm_T(src, src_res, gcol, dst, dst_res, banks, xn, junk, tag, f32dst=None, f32res=None):
            for _ in norm_T_gen(src, src_res, gcol, dst, dst_res, banks, xn, (tag, 'xn'), junk, (tag, 'junk'), f32dst, f32res):
                pass

        def mixer(s, l):
            add('pool', lambda e: e.dma_start(out=Wmem, in_=w_mem_d[l].rearrange("(kc p) n -> p kc n", p=128)),
                writes=['Wmem'], dma=True, dsem='Wmem')
            add('pool', lambda e: e.dma_start(out=Win, in_=w_in_d[l].rearrange("(kc p) n -> p kc n", p=128)),
                writes=['Win'], dma=True, dsem='Win')
            add('pool', lambda e: e.dma_start(out=Wout, in_=w_out_d[l].rearrange("(kc p) n -> p kc n", p=128)),
                writes=['Wout'], dma=True, dsem='Wout')
            add('sp', lambda e: e.dma_start(out=memx, in_=mem_d[s].rearrange("(t p) d -> p t d", p=128)),
                writes=['memx'], dma=True, dsem='memx')
            add('pool', lambda e: e.memset(mvA[:, :, :, 64:65], 1.0), writes=['mvA'])
            for k in range(3):
                add('pool', lambda e, k=k: e.memset(Vaug[k][:, :, 64:65], 1.0), writes=[('Vaug', k)])
            add('pool', lambda e: e.memset(zt[0][:, :, 0:2], 0.0), writes=[('zt', 0)])
            for k in range(3):
                for kv in range(2):
                    add('pool', lambda e, k=k, kv=kv: e.memset(kz[k][kv][:], 0.0), writes=[('kz', k)])
            add('pool', lambda e: e.memset(mkz[:], 0.0), writes=['mkz'])

            for mt in range(2):
                norm_T(memx[:, mt, :], 'memx', gvec[:, GC_MEM + l * 8:GC_MEM + l * 8 + 8],
                       hmT[:, :, mt * 128:(mt + 1) * 128], 'hmT', (0, 1), xn_m, junk_m, 'm')
                for kc in range(8):
                    add('pe', lambda e, kc=kc, mt=mt: e.matmul(PB[2][:], lhsT=hmT[:, kc, mt * 128:(mt + 1) * 128], rhs=Wmem[:, kc, :],
                                                               start=(kc == 0), stop=(kc == 7)),
                        reads=['hmT', 'Wmem'], writes=[('pb', 2)])
                add('act', lambda e: e.activation(out=mks, in_=PB[2][:, 0:256], func=AF.Copy), reads=[('pb', 2)], writes=['mks'])
                add('act', lambda e, mt=mt: e.activation(out=mvA[:, mt, :, 0:64], in_=PB[2][:, 256:512].rearrange("p (h d) -> p h d", h=4), func=AF.Copy),
                    reads=[('pb', 2)], writes=['mvA'])
                add('dve', lambda e: e.tensor_tensor(out=mksq, in0=mks, in1=mks, op=ALU.mult), reads=['mks'], writes=['mksq'])
                add('dve', lambda e: e.reduce_sum(out=mss, in_=mksq.rearrange("p (h d) -> p h d", h=4), axis=AX.X), reads=['mksq'], writes=['mss'])
                add('dve', lambda e: e.tensor_scalar(out=mss2, in0=mss, scalar1=1.0 / 64, scalar2=EPS, op0=ALU.mult, op1=ALU.add),
                    reads=['mss'], writes=['mss2'])
                add('pool', lambda e: e.tensor_tensor(out=mrq, in0=mss2, in1=mhalf[:, 0:4], op=ALU.pow), reads=['mss2', 'mhalf'], writes=['mrq'])
                add('dve', lambda e: e.tensor_tensor(out=mkn.rearrange("p (h d) -> p h d", h=4), in0=mks.rearrange("p (h d) -> p h d", h=4),
                                                     in1=mrq.unsqueeze(2).broadcast_to([128, 4, 64]), op=ALU.mult),
                    reads=['mks', 'mrq'], writes=['mkn'])
                pq = PB[3][:].bitcast(BF16)
                for c in range(2):
                    add('pe', lambda e, c=c, pq=pq: e.transpose(out=pq[:, c * 128:(c + 1) * 128], in_=mkn[:, c * 128:(c + 1) * 128], identity=identb[:]),
                        reads=['mkn', 'identb'], writes=[('pb', 3)])
                for h in range(4):
                    c, hf = h // 2, h % 2
                    lo, hi = hf * 64, hf * 64 + 64
                    add('dve', lambda e, mt=mt, pq=pq, h=h, c=c, lo=lo, hi=hi: e.tensor_scalar(out=mkz[lo:hi, h, mt * 128:(mt + 1) * 128], in0=pq[lo:hi, c * 128:(c + 1) * 128],
                                                                                               scalar1=gvec[lo:hi, GC_QK + l * 8 + 7:GC_QK + l * 8 + 8], scalar2=None, op0=ALU.mult),
                        reads=[('pb', 3), 'gvec'], writes=['mkz'])

            if DEBUG_STAGE == 'mem':
                return
            ntl = NT if DEBUG_STAGE != 'tile1' else 2
            go = GC_OUT + l * 8
            cw = GC_CONV + l * 6

            def stage1(i):
                p = i % 2
                k3 = i % 3
                yield from norm_T_gen(X[:, i, :], ('x', i), gvec[:, GC_MIX + l * 8:GC_MIX + l * 8 + 8], hTt[p], ('hTt', p), (0, 1),
                                      xn_m, ('m', 'xn'), junk_m, ('m', 'junk'))
                for half in range(2):
                    for kc in range(8):
                        add('pe', lambda e, kc=kc, half=half, p=p: e.matmul(PB[2 + half][:], lhsT=hTt[p][:, kc, :], rhs=Win[:, kc, half * 512:(half + 1) * 512],
                                                                          start=(kc == 0), stop=(kc == 7)),
                            reads=[('hTt', p), 'Win'], writes=[('pb', 2 + half)])
                yield
                for cc in range(6):
                    b = cc // 4
                    for kc in range(8):
                        add('pe', lambda e, kc=kc, cc=cc, b=b, p=p: e.matmul(PB[b][:, (cc % 4) * 128:(cc % 4 + 1) * 128],
                                                                           lhsT=Win[:, kc, 1024 + cc * 128:1024 + (cc + 1) * 128], rhs=hTt[p][:, kc, :],
                                                                           start=(kc == 0), stop=(kc == 7)),
                            reads=[('hTt', p), 'Win'], writes=[('pb', b)])
                yield
                add('act', lambda e: e.activation(out=qk[:, 0:512], in_=PB[2][:], func=AF.Copy), reads=[('pb', 2)], writes=['qk'])
                add('act', lambda e: e.activation(out=qk[:, 512:896], in_=PB[3][:, 0:384], func=AF.Copy), reads=[('pb', 3)], writes=['qk'])
                add('act', lambda e, k3=k3: e.activation(out=Vaug[k3][:, :, 0:64], in_=PB[3][:, 384:512].rearrange("p (h d) -> p h d", h=2), func=AF.Copy),
                    reads=[('pb', 3)], writes=[('Vaug', k3)])
                pcb = PB[0][:, 0:256].rearrange("p (c t) -> p c t", c=2)
                pcc = PB[0][:, 256:512].rearrange("p (c t) -> p c t", c=2)
                pcu = PB[1][:, 0:256].rearrange("p (c t) -> p c t", c=2)
                z = zt[p]
                zn = zt[1 - p]
                add('act', lambda e, pcc=pcc: e.activation(out=ccs, in_=pcc, func=AF.Copy), reads=[('pb', 0)], writes=['ccs'])
                yield
                add('dve', lambda e: e.tensor_tensor(out=qsq, in0=qk, in1=qk, op=ALU.mult), reads=['qk'], writes=['qsq'])
                add('dve', lambda e: e.reduce_sum(out=ssq, in_=qsq.rearrange("p (h d) -> p h d", h=14), axis=AX.X), reads=['qsq'], writes=['ssq'])
                add('dve', lambda e: e.tensor_scalar(out=ssq2, in0=ssq, scalar1=1.0 / 64, scalar2=EPS, op0=ALU.mult, op1=ALU.add),
                    reads=['ssq'], writes=['ssq2'])
                yield
                add('pool', lambda e: e.tensor_tensor(out=rq, in0=ssq2, in1=mhalf[:, 0:14], op=ALU.pow), reads=['ssq2', 'mhalf'], writes=['rq'])
                add('dve', lambda e, z=z, pcu=pcu: e.tensor_tensor(out=z[:, :, 2:130], in0=ccs, in1=pcu, op=ALU.mult),
                    reads=['ccs', ('pb', 1)], writes=[('zt', p)])
                yield
                add('pool', lambda e, z=z, zn=zn: e.tensor_copy(out=zn[:, :, 0:2], in_=z[:, :, 128:130]), reads=[('zt', p)], writes=[('zt', 1 - p)])
                add('dve', lambda e: e.tensor_tensor(out=qkn.rearrange("p (h d) -> p h d", h=14), in0=qk.rearrange("p (h d) -> p h d", h=14),
                                                     in1=rq.unsqueeze(2).broadcast_to([128, 14, 64]), op=ALU.mult),
                    reads=['qk', 'rq'], writes=['qkn'])
                yield
                pq = PB[2][:].bitcast(BF16)
                for c in range(7):
                    add('pe', lambda e, c=c, pq=pq: e.transpose(out=pq[:, c * 128:(c + 1) * 128], in_=qkn[:, c * 128:(c + 1) * 128], identity=identb[:]),
                        reads=['qkn', 'identb'], writes=[('pb', 2)])
                for c in range(2):
                    add('dve', lambda e, z=z, c=c: e.tensor_scalar(out=cacc[:, c, :], in0=z[:, c, 0:128], scalar1=gvec[:, cw + c:cw + c + 1], scalar2=None, op0=ALU.mult),
                        reads=[('zt', p), 'gvec'], writes=['cacc'])
                    for j in (1, 2):
                        add('dve', lambda e, z=z, c=c, j=j: e.scalar_tensor_tensor(out=cacc[:, c, :], in0=z[:, c, j:j + 128], scalar=gvec[:, cw + 2 * j + c:cw + 2 * j + c + 1],
                                                                                 in1=cacc[:, c, :], op0=ALU.mult, op1=ALU.add),
                            reads=[('zt', p), 'gvec', 'cacc'], writes=['cacc'])
                yield
                add('dve', lambda e, p=p, pq=pq: e.tensor_tensor(out=qTt[p], in0=pq[:, 0:896].rearrange("p (c t) -> p c t", c=7),
                                                                 in1=gvec[:, GC_QK + l * 8:GC_QK + l * 8 + 7].unsqueeze(2).broadcast_to([128, 7, 128]), op=ALU.mult),
                    reads=[('pb', 2), 'gvec'], writes=[('qTt', p)])
                for kv in range(2):
                    lo, hi = kv * 64, kv * 64 + 64
                    add('dve', lambda e, k3=k3, pq=pq, kv=kv, lo=lo, hi=hi: e.tensor_scalar(out=kz[k3][kv][lo:hi, :], in0=pq[lo:hi, 512:640],
                                                                                            scalar1=gvec[lo:hi, GC_QK + l * 8 + 4:GC_QK + l * 8 + 5], scalar2=None, op0=ALU.mult),
                        reads=[('pb', 2), 'gvec'], writes=[('kz', k3)])
                yield
                add('dve', lambda e, pcb=pcb: e.tensor_tensor(out=ycv, in0=cacc, in1=pcb, op=ALU.mult), reads=['cacc', ('pb', 0)], writes=['ycv'])
                add('dve', lambda e: e.tensor_tensor(out=csq, in0=ycv, in1=ycv, op=ALU.mult), reads=['ycv'], writes=['csq'])
                yield
                for c in range(2):
                    add('pe', lambda e, c=c: e.matmul(PB[1][:, 256:257], lhsT=csq[:, c, :], rhs=ones[:, 0:1], start=(c == 0), stop=(c == 1)),
                        reads=['ones', 'csq'], writes=[('pb', 1)])
                for c in range(2):
                    add('dve', lambda e, c=c, p=p: e.tensor_scalar(out=ycT[p][:, c, :], in0=ycv[:, c, :], scalar1=gvec[:, go + 4 + c:go + 5 + c], scalar2=None, op0=ALU.mult),
                        reads=['ycv', 'gvec'], writes=[('ycT', p)])
                yield
                add('dve', lambda e, p=p: e.tensor_scalar(out=rcs[p], in0=PB[1][:, 256:257], scalar1=1.0 / 256, scalar2=EPS, op0=ALU.mult, op1=ALU.add),
                    reads=[('pb', 1)], writes=[('rcs', p)])
                yield
                add('pool', lambda e, p=p: e.tensor_tensor(out=rcinv[p], in0=rcs[p], in1=mhalf[:, 0:1], op=ALU.pow), reads=[('rcs', p), 'mhalf'], writes=[('rcinv', p)])
                yield

            def stage2(i):
                p = i % 2
                k3 = i % 3
                k3p = (i - 1) % 3
                rels = [1] if i == 0 else [0, 1]
                blk = 0
                for kvh in range(2):
                    for r in rels:
                        kk = k3 if r == 1 else k3p
                        b = 4 + blk % 2
                        eb = blk % 2
                        add('pe', lambda e, kk=kk, p=p, kvh=kvh, b=b: e.matmul(PB[b][:], lhsT=kz[kk][kvh][:], rhs=qTt[p][:, 0:4, :], start=True, stop=True),
                            reads=[('kz', kk), ('qTt', p)], writes=[('pb', b)])
                        yield
                        add('act', lambda e, b=b, eb=eb: e.activation(out=es[eb], in_=PB[b][:], func=AF.Exp, scale=0.125),
                            reads=[('pb', b)], writes=[('es', eb)])
                        yield
                        add('dve', lambda e, eb=eb, r=r, kvh=kvh: e.tensor_tensor(out=PT[:, kvh * 2 + r, :].rearrange("p (h q) -> p h q", h=4),
                                                                                  in0=es[eb].rearrange("p (h q) -> p h q", h=4),
                                                                                  in1=ET[:, r, kvh * 4:kvh * 4 + 4, :], op=ALU.mult),
                            reads=[('es', eb), 'ET'], writes=[('PT', kvh * 2 + r)])
                        blk += 1
                yield
                for h in range(8):
                    kvh, g = h // 4, h % 4
                    b = 6 + kvh
                    po = PB[b][:, 0:260].rearrange("p (h d) -> p h d", h=4)
                    for ri, r in enumerate(rels):
                        kk = k3 if r == 1 else k3p
                        lastr = (ri == len(rels) - 1)
                        add('pe', lambda e, po=po, g=g, kvh=kvh, r=r, kk=kk, ri=ri, lastr=lastr: e.matmul(po[:, g, :], lhsT=PT[:, kvh * 2 + r, g * 128:(g + 1) * 128],
                                                                                                        rhs=Vaug[kk][:, kvh, :], start=(ri == 0), stop=lastr),
                            reads=[('PT', kvh * 2 + r), ('Vaug', kk)], writes=[('pb', b)])
                yield
                for mt in range(2):
                    b = 4 + mt
                    for h in range(4):
                        c = h // 2
                        add('pe', lambda e, b=b, h=h, c=c, mt=mt, p=p: e.matmul(PB[b][:, h * 128:(h + 1) * 128], lhsT=mkz[:, h, mt * 128:(mt + 1) * 128],
                                                                              rhs=qTt[p][:, 5 + c, :], start=True, stop=True),
                            reads=['mkz', ('qTt', p)], writes=[('pb', b)])
                    yield
                    add('act', lambda e, b=b, mt=mt: e.activation(out=PmT[:, mt, :], in_=PB[b][:], func=AF.Exp, scale=0.125),
                        reads=[('pb', b)], writes=[('PmT', mt)])
                for kvh in range(2):
                    po = PB[6 + kvh][:, 0:260].rearrange("p (h d) -> p h d", h=4)
                    add('dve', lambda e, po=po, kvh=kvh: e.tensor_tensor(out=den[:, kvh * 4:kvh * 4 + 4], in0=po[:, :, 64],
                                                                         in1=esink[:, l * 8 + kvh * 4:l * 8 + kvh * 4 + 4], op=ALU.add),
                        reads=[('pb', 6 + kvh), 'esink'], writes=['den'])
                yield
                add('dve', lambda e: e.reciprocal(out=rden, in_=den), reads=['den'], writes=['rden'])
                yield
                for kvh in range(2):
                    po = PB[6 + kvh][:, 0:260].rearrange("p (h d) -> p h d", h=4)
                    add('dve', lambda e, po=po, kvh=kvh: e.tensor_tensor(out=ysw[:, kvh * 256:(kvh + 1) * 256].rearrange("p (h d) -> p h d", h=4), in0=po[:, :, 0:64],
                                                                         in1=rden[:, kvh * 4:kvh * 4 + 4].unsqueeze(2).broadcast_to([128, 4, 64]), op=ALU.mult),
                        reads=[('pb', 6 + kvh), 'rden'], writes=['ysw'])
                pom = PB[4][:, 0:260].rearrange("p (h d) -> p h d", h=4)
                for h in range(4):
                    for mt in range(2):
                        add('pe', lambda e, h=h, mt=mt, pom=pom: e.matmul(pom[:, h, :], lhsT=PmT[:, mt, h * 128:(h + 1) * 128], rhs=mvA[:, mt, h, :],
                                                                         start=(mt == 0), stop=(mt == 1)),
                            reads=[('PmT', mt), 'mvA'], writes=[('pb', 4)])
                yield
                add('act', lambda e: e.activation(out=junk2[:, 0:512], in_=ysw, func=AF.Square, accum_out=oss[:, 0:1]),
                    reads=['ysw'], writes=['junk2', 'oss'])
                add('dve', lambda e, pom=pom: e.reciprocal(out=rdm, in_=pom[:, :, 64]), reads=[('pb', 4)], writes=['rdm'])
                yield
                add('dve', lambda e, pom=pom: e.tensor_tensor(out=ym.rearrange("p (h d) -> p h d", h=4), in0=pom[:, :, 0:64],
                                                              in1=rdm.unsqueeze(2).broadcast_to([128, 4, 64]), op=ALU.mult),
                    reads=[('pb', 4), 'rdm'], writes=['ym'])
                yield
                add('act', lambda e: e.activation(out=junk2[:, 512:768], in_=ym, func=AF.Square, accum_out=oss[:, 1:2]),
                    reads=['ym'], writes=['junk2', 'oss'])
                yield
                add('dve', lambda e: e.tensor_tensor(out=oss2, in0=oss, in1=oscale, op=ALU.mult), reads=['oss', 'oscale'], writes=['oss2'])
                add('dve', lambda e: e.tensor_scalar(out=oss3, in0=oss2, scalar1=EPS, scalar2=None, op0=ALU.add), reads=['oss2'], writes=['oss3'])
                yield
                add('pool', lambda e: e.tensor_tensor(out=orin, in0=oss3, in1=mhalf[:, 0:2], op=ALU.pow), reads=['oss3', 'mhalf'], writes=['orin'])
                yield
                add('act', lambda e: e.activation(out=ynb[:, 0:512], in_=ysw, func=AF.Copy, scale=orin[:, 0:1]), reads=['ysw', 'orin'], writes=['ynb'])
                add('act', lambda e: e.activation(out=ynb[:, 512:768], in_=ym, func=AF.Copy, scale=orin[:, 1:2]), reads=['ym', 'orin'], writes=['ynb'])
                yield
                pyt = PB[5][:].bitcast(BF16)
                for c in range(6):
                    add('pe', lambda e, c=c, pyt=pyt: e.transpose(out=pyt[:, c * 128:(c + 1) * 128], in_=ynb[:, c * 128:(c + 1) * 128], identity=identb[:]),
                        reads=['ynb', 'identb'], writes=[('pb', 5)])
                yield
                add('dve', lambda e, pyt=pyt: e.tensor_tensor(out=yT[:, 0:4, :], in0=pyt[:, 0:512].rearrange("p (c t) -> p c t", c=4),
                                                              in1=gvec[:, go:go + 4].unsqueeze(2).broadcast_to([128, 4, 128]), op=ALU.mult),
                    reads=[('pb', 5), 'gvec'], writes=['yT'])
                add('dve', lambda e, pyt=pyt: e.tensor_tensor(out=yT[:, 6:8, :], in0=pyt[:, 512:768].rearrange("p (c t) -> p c t", c=2),
                                                              in1=gvec[:, go + 6:go + 8].unsqueeze(2).broadcast_to([128, 2, 128]), op=ALU.mult),
                    reads=[('pb', 5), 'gvec'], writes=['yT'])
                yield
                for half in range(2):
                    b = 6 + half
                    main = [0, 1, 2, 3, 6, 7]
                    for n_, kc in enumerate(main):
                        add('pe', lambda e, kc=kc, half=half, b=b, n_=n_: e.matmul(PB[b][:], lhsT=yT[:, kc, :], rhs=Wout[:, kc, half * 512:(half + 1) * 512],
                                                                                 start=(n_ == 0), stop=(n_ == 5)),
                            reads=['yT', 'Wout'], writes=[('pb', b)])
                    bc_ = 4 + half
                    for c in range(2):
                        add('pe', lambda e, c=c, half=half, bc_=bc_, p=p: e.matmul(PB[bc_][:], lhsT=ycT[p][:, c, :], rhs=Wout[:, 4 + c, half * 512:(half + 1) * 512],
                                                                                 start=(c == 0), stop=(c == 1)),
                            reads=[('ycT', p), 'Wout'], writes=[('pb', bc_)])
                    yield
                    xs = X[:, i, half * 512:(half + 1) * 512]
                    add('dve', lambda e, b=b, xs=xs: e.tensor_tensor(out=xs, in0=PB[b][:], in1=xs, op=ALU.add),
                        reads=[('pb', b), ('x', i)], writes=[('x', i)])
                    add('dve', lambda e, bc_=bc_, xs=xs, p=p: e.scalar_tensor_tensor(out=xs, in0=PB[bc_][:], scalar=rcinv[p], in1=xs, op0=ALU.mult, op1=ALU.add),
                        reads=[('pb', bc_), ('x', i), ('rcinv', p)], writes=[('x', i)])
                yield

            for step in range(ntl + 1):
                gens = []
                if step < ntl:
                    gens.append(stage1(step))
                if step >= 1:
                    gens.append(stage2(step - 1))
                round_robin(gens)

        gcount = [0]

        def ffn(s, l):
            moe = (l % 2 == 1)
            li = l // 2
            xn_par = [xn_f, AT[1].rearrange("p a b -> p (a b)").bitcast(F32)]
            xn_res = [('f', 'xn'), ('AT', 1)]
            jflat = AT[0].rearrange("p a b -> p (a b)")
            junk_par = [jflat[:, 0:1024], jflat[:, 1024:2048]]

            def ffn_norm_tile(i):
                k = i % 2
                yield from norm_T_gen(X[:, i, :], ('x', i), gvec[:, GC_FFN + l * 8:GC_FFN + l * 8 + 8], hT[:, :, i * 128:(i + 1) * 128], 'hT',
                                      (2 * k, 2 * k + 1), xn_par[k], xn_res[k], junk_par[k], ('AT', 0),
                                      f32dst=(h2f[k] if moe else None), f32res=('h2f', k))
                if moe:
                    for kc in range(8):
                        add('pe', lambda e, kc=kc, k=k: e.matmul(PB[4 + k][:, 0:8], lhsT=h2f[k][:, kc, :], rhs=wr[:, li, kc, :], start=(kc == 0), stop=(kc == 7)),
                            reads=[('h2f', k), 'wr'], writes=[('pb', 4 + k)])
                    yield
                    add('dve', lambda e, i=i, k=k: e.tensor_tensor(out=L[:, i, :], in0=PB[4 + k][:, 0:8], in1=brow[:, BR_BR + li * 8:BR_BR + li * 8 + 8], op=ALU.add),
                        reads=[('pb', 4 + k), 'brow'], writes=['L'])
                    yield

            for i in range(0, NT, 2):
                round_robin([ffn_norm_tile(i), ffn_norm_tile(i + 1)])
            if moe:
                bc = lambda a: a.unsqueeze(2).broadcast_to([128, NT, 8])
                add('dve', lambda e: e.reduce_max(out=m1, in_=L, axis=AX.X), reads=['L'], writes=['m1'])
                add('dve', lambda e: e.tensor_tensor(out=eq1, in0=L, in1=bc(m1), op=ALU.is_equal), reads=['L', 'm1'], writes=['eq1'])
                add('dve', lambda e: e.scalar_tensor_tensor(out=L2.rearrange("p a b -> p (a b)"), in0=eq1.rearrange("p a b -> p (a b)"), scalar=-1e30,
                                                            in1=L.rearrange("p a b -> p (a b)"), op0=ALU.mult, op1=ALU.add),
                    reads=['eq1', 'L'], writes=['L2'])
                add('dve', lambda e: e.reduce_max(out=m2, in_=L2, axis=AX.X), reads=['L2'], writes=['m2'])
                add('dve', lambda e: e.tensor_tensor(out=eq2, in0=L2, in1=bc(m2), op=ALU.is_equal), reads=['L2', 'm2'], writes=['eq2'])
                add('dve', lambda e: e.tensor_tensor(out=dd, in0=m2, in1=m1, op=ALU.subtract), reads=['m1', 'm2'], writes=['dd'])
                add('act', lambda e: e.activation(out=ee, in_=dd, func=AF.Exp), reads=['dd'], writes=['ee'])
                add('dve', lambda e: e.tensor_scalar(out=g1, in0=ee, scalar1=1.0, scalar2=None, op0=ALU.add), reads=['ee'], writes=['g1'])
                add('dve', lambda e: e.reciprocal(out=g1, in_=g1), reads=['g1'], writes=['g1'])
                add('dve', lambda e: e.tensor_tensor(out=g2, in0=ee, in1=g1, op=ALU.mult), reads=['ee', 'g1'], writes=['g2'])
                add('dve', lambda e: e.tensor_tensor(out=eq1, in0=eq1, in1=bc(g1), op=ALU.mult), reads=['eq1', 'g1'], writes=['eq1'])
                add('dve', lambda e: e.tensor_tensor(out=eq2, in0=eq2, in1=bc(g2), op=ALU.mult), reads=['eq2', 'g2'], writes=['eq2'])
                add('dve', lambda e: e.tensor_tensor(out=comb, in0=eq1, in1=eq2, op=ALU.add), reads=['eq1', 'eq2'], writes=['comb'])

            experts = list(range(N_EXP)) if moe else [None]
            groups = [(ex, g) for ex in experts for g in range(NG)]

            def issue_dma(ex, g):
                gi = gcount[0]
                gcount[0] += 1
                b = gi % NWB
                Wg_, Wu_, Wd_ = Wb[b]
                if ex is None:
                    sg_, su_, sd_ = wgd_d[li], wud_d[li], wdd_d[li]
                else:
                    sg_, su_, sd_ = wgm_d[li, ex], wum_d[li, ex], wdm_d[li, ex]
                add('pool', lambda e: e.dma_start(out=Wg_, in_=sg_[:, g * GW:(g + 1) * GW].rearrange("(kc p) n -> p kc n", p=128)),
                    writes=[('Wg', b)], dma=True, dsem=('Wg', b))
                add('pool', lambda e: e.dma_start(out=Wu_, in_=su_[:, g * GW:(g + 1) * GW].rearrange("(kc p) n -> p kc n", p=128)),
                    writes=[('Wu', b)], dma=True, dsem=('Wu', b))
                add('pool', lambda e: e.dma_start(out=Wd_, in_=sd_[g * GW:(g + 1) * GW, :].rearrange("(fc p) n -> p fc n", p=128)),
                    writes=[('Wd', b)], dma=True, dsem=('Wd', b))
                return b

            bufs = {}
            PRE = NWB - 1
            for k in range(min(PRE, len(groups))):
                bufs[k] = issue_dma(*groups[k])
            pending = [None]
            step = [0]
            dcount = [0]

            def do_down(ex, b, tg, ap):
                Wd_ = Wb[b][2]
                for tt in range(4):
                    tile = tg * 4 + tt
                    for half in range(2):
                        db = 4 + dcount[0] % 4
                        dcount[0] += 1
                        for fcl in range(GW // 128):
                            add('pe', lambda e, db=db, fcl=fcl, tt=tt, half=half: e.matmul(PB[db][:], lhsT=AT[ap][:, fcl, tt * 128:(tt + 1) * 128],
                                                                                         rhs=Wd_[:, fcl, half * 512:(half + 1) * 512],
                                                                                         start=(fcl == 0), stop=(fcl == GW // 128 - 1)),
                                reads=[('AT', ap), ('Wd', b)], writes=[('pb', db)])
                        xs = X[:, tile, half * 512:(half + 1) * 512]
                        if ex is None:
                            add('dve', lambda e, db=db, xs=xs: e.tensor_tensor(out=xs, in0=PB[db][:], in1=xs, op=ALU.add),
                                reads=[('pb', db), ('x', tile)], writes=[('x', tile)])
                        else:
                            add('dve', lambda e, db=db, xs=xs, tile=tile: e.scalar_tensor_tensor(out=xs, in0=PB[db][:], scalar=comb[:, tile, ex:ex + 1], in1=xs,
                                                                                               op0=ALU.mult, op1=ALU.add),
                                reads=[('pb', db), ('x', tile), 'comb'], writes=[('x', tile)])

            for k, (ex, g) in enumerate(groups):
                b = bufs[k]
                Wg_, Wu_, _ = Wb[b]
                for tg in range(4):
                    ap = step[0] % 2
                    step[0] += 1
                    for fcl in range(GW // 128):
                        gb = fcl % 2
                        ub = 2 + fcl % 2
                        for kc in range(8):
                            add('pe', lambda e, kc=kc, fcl=fcl, gb=gb, tg=tg, Wg_=Wg_: e.matmul(PB[gb][:], lhsT=Wg_[:, kc, fcl * 128:(fcl + 1) * 128], rhs=hT[:, kc, tg * 512:(tg + 1) * 512],
                                                                                     start=(kc == 0), stop=(kc == 7)),
                                reads=[('Wg', b), 'hT'], writes=[('pb', gb)])
                        for kc in range(8):
                            add('pe', lambda e, kc=kc, fcl=fcl, ub=ub, tg=tg, Wu_=Wu_: e.matmul(PB[ub][:], lhsT=Wu_[:, kc, fcl * 128:(fcl + 1) * 128], rhs=hT[:, kc, tg * 512:(tg + 1) * 512],
                                                                                     start=(kc == 0), stop=(kc == 7)),
                                reads=[('Wu', b), 'hT'], writes=[('pb', ub)])
                        add('act', lambda e, gb=gb: e.activation(out=sg[gb], in_=PB[gb][:], func=AF.Silu), reads=[('pb', gb)], writes=[('sg', gb)])
                        add('dve', lambda e, gb=gb, ub=ub, fcl=fcl, ap=ap: e.tensor_tensor(out=AT[ap][:, fcl, :], in0=sg[gb], in1=PB[ub][:], op=ALU.mult),
                            reads=[('sg', gb), ('pb', ub)], writes=[('AT', ap)])
                    if pending[0] is not None:
                        do_down(*pending[0])
                    pending[0] = (ex, b, tg, ap)
                    if tg == 0 and k + PRE < len(groups):
                        bufs[k + PRE] = issue_dma(*groups[k + PRE])
            do_down(*pending[0])

        for s in range(n_seq):
            for q4 in range(4):
                add('sp', lambda e, q4=q4, s=s: e.dma_start(out=X[:, q4 * 4:(q4 + 1) * 4, :], in_=x_d[s, q4 * 512:(q4 + 1) * 512, :].rearrange("(t p) d -> p t d", p=128)),
                    writes=[('x', q4 * 4 + t) for t in range(4)], dma=True, dsem=('xin', q4))
            for l in layers:
                S.barrier()
                mixer(s, l)
                S.barrier()
                if DEBUG_STAGE in ('mem', 'tile1', 'mixer'):
                    continue
                ffn(s, l)
            for q4 in range(4):
                add('sp', lambda e, q4=q4, s=s: e.dma_start(out=out_d[s, q4 * 512:(q4 + 1) * 512, :].rearrange("(t p) d -> p t d", p=128), in_=X[:, q4 * 4:(q4 + 1) * 4, :]),
                    reads=[('x', q4 * 4 + t) for t in range(4)], dma=True, dsem=('xout', q4))
        S.emit_all()
    return nc


def _alibi_table():
    j = np.arange(128)[:, None, None, None].astype(np.float64)
    r = np.arange(2)[None, :, None, None]
    h = np.arange(8)[None, None, :, None]
    i = np.arange(128)[None, None, None, :].astype(np.float64)
    kpos = j + (r - 1) * 128
    kc = np.floor(kpos / 64)
    qc = np.floor(i / 64)
    valid = (kc >= qc - 2) & (kc <= qc)
    slope = 2.0 ** (-(h + 1.0))
    t = np.where(valid, np.exp(-slope * np.abs(i - kpos)), 0.0)
    return np.ascontiguousarray(t.reshape(128, 2 * 8 * 128)).astype(np.float32)


def _prep_small(inp):
    f = lambda a: np.asarray(a, dtype=np.float32)
    gvec = np.zeros((128, GC_N), np.float32)
    g_out = np.concatenate([f(inp['g_out_swa']), f(inp['g_out_conv']), f(inp['g_out_mem'])], axis=1)
    for l in range(DEPTH):
        gvec[:, GC_MIX + l * 8:GC_MIX + l * 8 + 8] = f(inp['g_mix'])[l].reshape(8, 128).T
        gvec[:, GC_FFN + l * 8:GC_FFN + l * 8 + 8] = f(inp['g_ffn'])[l].reshape(8, 128).T
        gvec[:, GC_MEM + l * 8:GC_MEM + l * 8 + 8] = f(inp['g_mem'])[l].reshape(8, 128).T
        gvec[:, GC_OUT + l * 8:GC_OUT + l * 8 + 8] = g_out[l].reshape(8, 128).T
        gq = np.tile(f(inp['g_q_swa'])[l], 2)
        gk = np.tile(f(inp['g_k_swa'])[l], 2)
        gqm = np.tile(f(inp['g_q_mem'])[l], 2)
        gkm = np.tile(f(inp['g_k_mem'])[l], 2)
        for c in range(4):
            gvec[:, GC_QK + l * 8 + c] = gq
        gvec[:, GC_QK + l * 8 + 4] = gk
        gvec[:, GC_QK + l * 8 + 5] = gqm
        gvec[:, GC_QK + l * 8 + 6] = gqm
        gvec[:, GC_QK + l * 8 + 7] = gkm
        cw = f(inp['conv_w'])[l]
        for j in range(3):
            for c in range(2):
                gvec[:, GC_CONV + l * 6 + 2 * j + c] = cw[j, c * 128:(c + 1) * 128]
    brow = np.zeros((128, BR_N), np.float32)
    brow[:, BR_SINK:BR_SINK + 32] = f(inp['sinks']).reshape(1, 32)
    brow[:, BR_BR:BR_BR + 16] = f(inp['b_router']).reshape(1, 16)
    wrr = f(inp['w_router']).reshape(2, 8, 128, 8).transpose(2, 0, 1, 3).reshape(128, 128)
    return gvec, brow, np.ascontiguousarray(wrr)


def _perm_w_in(w_in):
    heads = [0, 4, 1, 5, 2, 6, 3, 7]
    cols = []
    for h in heads:
        cols += list(range(h * 64, (h + 1) * 64))
    cols += list(range(512, 640))
    cols += list(range(1536, 1792))
    cols += list(range(640, 768))
    cols += list(range(768, 1536))
    return np.ascontiguousarray(np.asarray(w_in, dtype=np.float32)[:, :, cols])


def kernel(**inp):
    n_cores = 8
    x = np.asarray(inp['x'], dtype=np.float32)
    mem = np.asarray(inp['mem'], dtype=np.float32)
    B = x.shape[0]
    n_seq = B // n_cores
    gvec, brow, wrr = _prep_small(inp)
    shared = {
        "w_in": _perm_w_in(inp['w_in']),
        "w_mem_kv": np.asarray(inp['w_mem_kv'], dtype=np.float32),
        "w_out": np.asarray(inp['w_out'], dtype=np.float32),
        "w_gate_dense": np.asarray(inp['w_gate_dense'], dtype=np.float32),
        "w_up_dense": np.asarray(inp['w_up_dense'], dtype=np.float32),
        "w_down_dense": np.asarray(inp['w_down_dense'], dtype=np.float32),
        "w_gate_moe": np.asarray(inp['w_gate_moe'], dtype=np.float32),
        "w_up_moe": np.asarray(inp['w_up_moe'], dtype=np.float32),
        "w_down_moe": np.asarray(inp['w_down_moe'], dtype=np.float32),
        "gvec": gvec, "brow": brow, "wr": wrr, "et": _alibi_table(),
        "ident": np.eye(128, dtype=np.float32),
    }
    nc = build_program(layers=tuple(range(DEPTH)), n_seq=n_seq)
    in_maps = []
    for c in range(n_cores):
        m = dict(shared)
        m["x"] = np.ascontiguousarray(x[c * n_seq:(c + 1) * n_seq])
        m["mem"] = np.ascontiguousarray(mem[c * n_seq:(c + 1) * n_seq])
        in_maps.append(m)
    res = run_bass_kernel_spmd(nc, in_maps, core_ids=list(range(n_cores)))
    out = np.concatenate([np.asarray(r["out"]) for r in res.results], axis=0)
    return out.astype(np.float32)
```

```python
import contextlib
import numpy as np
import concourse.bass as bass
import concourse.mybir as mybir
from concourse.bass_utils import run_bass_kernel_spmd

F32 = mybir.dt.float32
BF16 = mybir.dt.bfloat16
AF = mybir.ActivationFunctionType
ALU = mybir.AluOpType
AX = mybir.AxisListType

D_MODEL = 1024
SEQ = 2048
NT = SEQ // 128
DEPTH = 4
D_FF = 3584
N_EXP = 8
MEM_LEN = 256
EPS = 1e-6
GW = 512
NG = D_FF // GW
NWB = 3

ENGS = ['pe', 'act', 'dve', 'pool', 'sp']
DEBUG_STAGE = None
DEBUG_LEVEL = 99


class Op:
    __slots__ = ('eng', 'emit', 'deps', 'raw', 'is_dma', 'dsem', 'dval', 'sig', 'cnt', 'idx')


class Sched:
    def __init__(self, nc):
        self.nc = nc
        self.ops = []
        self.per_eng = {e: [] for e in ENGS}
        self.last_w = {}
        self.readers = {}
        self.dma_val = {}
        self.pending_bar = {}

    def add(self, eng, emit, reads=(), writes=(), dma=False, dsem=None):
        op = Op()
        op.eng = eng; op.emit = emit; op.is_dma = dma; op.idx = len(self.ops)
        op.sig = False; op.cnt = 0; op.dsem = None; op.dval = 0
        deps = set()
        for r in reads:
            w = self.last_w.get(r)
            if w is not None:
                deps.add(w)
        op.raw = set(deps)
        for r in writes:
            w = self.last_w.get(r)
            if w is not None:
                deps.add(w)
            for rd in self.readers.get(r, ()):
                deps.add(rd)
        pb = self.pending_bar.pop(eng, None)
        if pb:
            deps |= pb
        for r in reads:
            self.readers.setdefault(r, []).append(op.idx)
        for r in writes:
            self.last_w[r] = op.idx
            self.readers[r] = []
        if dma:
            v = self.dma_val.get(dsem, 0) + 16
            self.dma_val[dsem] = v
            op.dsem = dsem; op.dval = v
        op.deps = deps
        self.ops.append(op)
        self.per_eng[eng].append(op)
        return op

    def barrier(self):
        last = set()
        for e in ENGS:
            if self.per_eng[e]:
                last.add(self.per_eng[e][-1].idx)
        for e in ENGS:
            self.pending_bar[e] = set(last)

    def finalize(self):
        ops = self.ops
        for op in ops:
            for d in op.deps:
                dop = ops[d]
                if dop.is_dma:
                    continue
                if dop.eng == 'pe' and op.eng == 'pe' and not op.is_dma:
                    continue
                if dop.eng == op.eng and not op.is_dma and d not in op.raw:
                    continue
                dop.sig = True
        for e in ENGS:
            c = 0
            for op in self.per_eng[e]:
                if op.sig and not op.is_dma:
                    c += 1
                op.cnt = c

    def emit_all(self, final_waits_eng='sp'):
        nc = self.nc
        self.finalize()
        with contextlib.ExitStack() as st:
            esem = {e: st.enter_context(nc.semaphore('s_' + e)) for e in ENGS}
            dsem = {k: st.enter_context(nc.semaphore('d_%d' % i)) for i, k in enumerate(self.dma_val)}
            block = st.enter_context(nc.Block())
            ops = self.ops

            def run(e, eng):
                waited = {}
                for op in self.per_eng[e]:
                    for d in sorted(op.deps):
                        dop = ops[d]
                        if dop.is_dma:
                            key = ('d', dop.dsem); sem = dsem[dop.dsem]; val = dop.dval
                        else:
                            if dop.eng == 'pe' and e == 'pe' and not op.is_dma:
                                continue
                            if dop.eng == e and not op.is_dma and d not in op.raw:
                                continue
                            key = ('e', dop.eng); sem = esem[dop.eng]; val = dop.cnt
                        if waited.get(key, 0) >= val:
                            continue
                        waited[key] = val
                        eng.wait_ge(sem, val)
                    ins = op.emit(eng)
                    if op.is_dma:
                        ins.then_inc(dsem[op.dsem], 16)
                    elif op.sig:
                        ins.then_inc(esem[e], 1)
                if e == final_waits_eng:
                    for k, v in self.dma_val.items():
                        if waited.get(('d', k), 0) < v:
                            eng.wait_ge(dsem[k], v)

            @block.tensor
            def _(eng):
                run('pe', eng)

            @block.scalar
            def _(eng):
                run('act', eng)

            @block.vector
            def _(eng):
                run('dve', eng)

            @block.gpsimd
            def _(eng):
                run('pool', eng)

            @block.sync
            def _(eng):
                run('sp', eng)


GC_MIX, GC_FFN, GC_MEM, GC_OUT, GC_QK, GC_CONV = 0, 32, 64, 96, 128, 160
GC_N = 184
BR_SINK, BR_BR = 0, 32
BR_N = 48


def build_program(layers=(0, 1, 2, 3), n_seq=2):
    nc = bass.Bass("TRN2", target_bir_lowering=False)

    def din(name, shape):
        return nc.dram_tensor(name, list(shape), F32, kind="ExternalInput").ap()

    x_d = din("x", [n_seq, SEQ, D_MODEL])
    mem_d = din("mem", [n_seq, MEM_LEN, D_MODEL])
    w_in_d = din("w_in", [DEPTH, D_MODEL, 1792])
    w_mem_d = din("w_mem_kv", [DEPTH, D_MODEL, 512])
    w_out_d = din("w_out", [DEPTH, D_MODEL, D_MODEL])
    wgd_d = din("w_gate_dense", [2, D_MODEL, D_FF])
    wud_d = din("w_up_dense", [2, D_MODEL, D_FF])
    wdd_d = din("w_down_dense", [2, D_FF, D_MODEL])
    wgm_d = din("w_gate_moe", [2, N_EXP, D_MODEL, D_FF])
    wum_d = din("w_up_moe", [2, N_EXP, D_MODEL, D_FF])
    wdm_d = din("w_down_moe", [2, N_EXP, D_FF, D_MODEL])
    gvec_d = din("gvec", [128, GC_N])
    brow_d = din("brow", [128, BR_N])
    wr_d = din("wr", [128, 128])
    et_d = din("et", [128, 2 * 8 * 128])
    ident_d = din("ident", [128, 128])
    out_d = nc.dram_tensor("out", [n_seq, SEQ, D_MODEL], F32, kind="ExternalOutput").ap()

    S = Sched(nc)
    with contextlib.ExitStack() as st:
        def sb(name, shape, dt):
            return st.enter_context(nc.sbuf_tensor(name, shape, dt))

        X = sb("X", [128, NT, D_MODEL], F32)
        gvec = sb("gvec_s", [128, GC_N], F32)
        brow = sb("brow_s", [128, BR_N], F32)
        esink = sb("esink", [128, 32], F32)
        wr = sb("wr_s", [128, 2, 8, 8], F32)
        ET = sb("ET", [128, 2, 8, 128], BF16)
        ident = sb("ident_s", [128, 128], F32)
        identb = sb("identb", [128, 128], BF16)
        ones = sb("ones_s", [128, 2], F32)
        mhalf = sb("mhalf", [128, 16], F32)
        small = sb("small", [128, 256], F32)
        UN = 66 * 1024
        U = sb("U", [128, UN], BF16)
        PB = [st.enter_context(nc.psum_tensor("pb%d" % k, [128, 512], F32)) for k in range(8)]

        class Carver:
            def __init__(self):
                self.off = 0

            def take(self, shape, dt):
                n = int(np.prod(shape[1:]))
                nb = n * (4 if dt == F32 else 2)
                nb = (nb + 63) // 64 * 64
                a = U[:, self.off // 2:(self.off + nb) // 2]
                self.off += nb
                assert self.off <= UN * 2, (self.off, UN * 2)
                if dt == F32:
                    a = a.bitcast(F32)
                a = a[:, 0:n]
                if len(shape) == 3:
                    a = a.rearrange("p (a b) -> p a b", a=shape[1])
                elif len(shape) == 4:
                    a = a.rearrange("p (a b c) -> p a b c", a=shape[1], b=shape[2])
                return a

        cm = Carver()
        Win = cm.take([128, 8, 1792], BF16)
        Wout = cm.take([128, 8, 1024], BF16)
        Wmem = cm.take([128, 8, 512], BF16)
        memx = cm.take([128, 2, 1024], F32)
        hmT = cm.take([128, 8, 256], BF16)
        mkT = cm.take([128, 2, 256], BF16)
        mvA = cm.take([128, 2, 4, 65], BF16)
        mks = cm.take([128, 256], F32)
        mksq = cm.take([128, 256], F32)
        mkn = cm.take([128, 256], BF16)
        xn_m = cm.take([128, 1024], F32)
        junk_m = cm.take([128, 1024], BF16)
        hTt = [cm.take([128, 8, 128], BF16) for _ in range(2)]
        qk = cm.take([128, 896], F32)
        qsq = cm.take([128, 896], F32)
        qkn = cm.take([128, 896], BF16)
        qTt = [cm.take([128, 7, 128], BF16) for _ in range(2)]
        Vaug = [cm.take([128, 2, 65], BF16) for _ in range(3)]
        kz = [[cm.take([128, 128], BF16) for _ in range(2)] for _ in range(3)]
        ycT = [cm.take([128, 2, 128], BF16) for _ in range(2)]
        junk2 = cm.take([128, 768], BF16)
        mkz = cm.take([128, 4, 256], BF16)
        es = [cm.take([128, 512], BF16) for _ in range(2)]
        PT = cm.take([128, 4, 512], BF16)
        PmT = cm.take([128, 2, 512], BF16)
        ysw = cm.take([128, 512], F32)
        ym = cm.take([128, 256], F32)
        ynb = cm.take([128, 768], BF16)
        yT = cm.take([128, 8, 128], BF16)
        ccs = cm.take([128, 2, 128], F32)
        zt = [cm.take([128, 2, 130], F32) for _ in range(2)]
        cacc = cm.take([128, 2, 128], F32)
        ycv = cm.take([128, 2, 128], F32)
        csq = cm.take([128, 2, 128], F32)
        rcv = cm.take([128, 128], F32)
        mixer_bytes = cm.off
        cf = Carver()
        hT = cf.take([128, 8, SEQ], BF16)
        Wb = [(cf.take([128, 8, GW], BF16), cf.take([128, 8, GW], BF16), cf.take([128, GW // 128, 1024], BF16))
              for _ in range(NWB)]
        AT = [cf.take([128, GW // 128, 512], BF16) for _ in range(2)]
        sg = [cf.take([128, 512], BF16) for _ in range(2)]
        xn_f = cf.take([128, 1024], F32)
        h2f = [cf.take([128, 8, 128], F32) for _ in range(2)]
        L = cf.take([128, NT, 8], F32)
        L2 = cf.take([128, NT, 8], F32)
        eq1 = cf.take([128, NT, 8], F32)
        eq2 = cf.take([128, NT, 8], F32)
        comb = cf.take([128, NT, 8], F32)
        ffn_bytes = cf.off
        if DEBUG_STAGE is not None:
            print('mixer_bytes', mixer_bytes, 'ffn_bytes', ffn_bytes, 'U bytes', UN * 2)

        sm_off = [0]

        def smalloc(n):
            a = small[:, sm_off[0]:sm_off[0] + n]
            sm_off[0] += n
            assert sm_off[0] <= 256
            return a

        ss_ = [smalloc(1) for _ in range(2)]
        ss2_ = [smalloc(1) for _ in range(2)]
        rinv_ = [smalloc(1) for _ in range(2)]
        ssq = smalloc(14); ssq2 = smalloc(14); rq = smalloc(14)
        mss = smalloc(4); mss2 = smalloc(4); mrq = smalloc(4)
        den = smalloc(8); rden = smalloc(8)
        denm = smalloc(4); rdm = smalloc(4)
        oss = smalloc(2); oss2 = smalloc(2); oss3 = smalloc(2); orin = smalloc(2)
        oscale = smalloc(2)
        rcs = [smalloc(1) for _ in range(2)]
        rcinv = [smalloc(1) for _ in range(2)]
        m1 = smalloc(16); m2 = smalloc(16); dd = smalloc(16); ee = smalloc(16); g1 = smalloc(16); g2 = smalloc(16)

        add = S.add
        nstat = [0]

        add('sp', lambda e: e.dma_start(out=gvec[:], in_=gvec_d), writes=['gvec'], dma=True, dsem='gvec')
        add('sp', lambda e: e.dma_start(out=brow[:], in_=brow_d), writes=['brow'], dma=True, dsem='brow')
        add('sp', lambda e: e.dma_start(out=wr[:], in_=wr_d.rearrange("p (m k e) -> p m k e", m=2, k=8)),
            writes=['wr'], dma=True, dsem='wr')
        add('pool', lambda e: e.dma_start(out=ET[:], in_=et_d.rearrange("p (r h q) -> p r h q", r=2, h=8)),
            writes=['ET'], dma=True, dsem='ET')
        add('sp', lambda e: e.dma_start(out=ident[:], in_=ident_d), writes=['ident'], dma=True, dsem='ident')
        add('pool', lambda e: e.memset(mhalf[:], -0.5), writes=['mhalf'])
        add('pool', lambda e: e.memset(ones[:], 1.0), writes=['ones'])
        add('pool', lambda e: e.memset(oscale[:, 0:1], 1.0 / 512), writes=['oscale'])
        add('pool', lambda e: e.memset(oscale[:, 1:2], 1.0 / 256), writes=['oscale'])
        add('act', lambda e: e.activation(out=identb[:], in_=ident[:], func=AF.Copy), reads=['ident'], writes=['identb'])
        add('act', lambda e: e.activation(out=esink[:], in_=brow[:, BR_SINK:BR_SINK + 32], func=AF.Exp),
            reads=['brow'], writes=['esink'])

        def round_robin(gens):
            gens = list(gens)
            while gens:
                for g in list(gens):
                    try:
                        next(g)
                    except StopIteration:
                        gens.remove(g)

        def norm_T_gen(src, src_res, gcol, dst, dst_res, banks, xn, xn_res, junk, junk_res, f32dst=None, f32res=None):
            k = nstat[0] % 2
            nstat[0] += 1
            ss, ss2, rinv = ss_[k], ss2_[k], rinv_[k]
            add('act', lambda e: e.activation(out=junk, in_=src, func=AF.Square, accum_out=ss),
                reads=[src_res], writes=[junk_res, ('ss', k)])
            yield
            add('pool', lambda e: e.tensor_scalar(out=ss2, in0=ss, scalar1=1.0 / D_MODEL, scalar2=EPS,
                                                  op0=ALU.mult, op1=ALU.add), reads=[('ss', k)], writes=[('ss2', k)])
            add('pool', lambda e: e.tensor_tensor(out=rinv, in0=ss2, in1=mhalf[:, 0:1], op=ALU.pow),
                reads=[('ss2', k), 'mhalf'], writes=[('rinv', k)])
            yield
            add('act', lambda e: e.activation(out=xn, in_=src, func=AF.Copy, scale=rinv),
                reads=[src_res, ('rinv', k)], writes=[xn_res])
            yield
            for kc in range(8):
                b = banks[kc // 4]
                add('pe', lambda e, kc=kc, b=b: e.transpose(out=PB[b][:, (kc % 4) * 128:(kc % 4 + 1) * 128],
                                                            in_=xn[:, kc * 128:(kc + 1) * 128], identity=ident[:]),
                    reads=[xn_res, 'ident'], writes=[('pb', b)])
            yield
            for j in range(2):
                b = banks[j]
                pin = PB[b][:].rearrange("p (a b) -> p a b", a=4)
                gin = gcol[:, 4 * j:4 * j + 4].unsqueeze(2).broadcast_to([128, 4, 128])
                if f32dst is not None:
                    add('dve', lambda e, j=j, pin=pin, gin=gin: e.tensor_tensor(out=f32dst[:, 4 * j:4 * j + 4, :], in0=pin, in1=gin, op=ALU.mult),
                        reads=[('pb', b), 'gvec'], writes=[f32res])
                add('dve', lambda e, j=j, pin=pin, gin=gin: e.tensor_tensor(out=dst[:, 4 * j:4 * j + 4, :], in0=pin, in1=gin, op=ALU.mult),
                    reads=[('pb', b), 'gvec'], writes=[dst_res])
            yield

        def norm_T(src, src_res, gcol, dst, dst_res, banks, xn, junk, tag, f32dst=None, f32res=None):
            for _ in norm_T_gen(src, src_res, gcol, dst, dst_res, banks, xn, (tag, 'xn'), junk, (tag, 'junk'), f32dst, f32res):
                pass

        def mixer(s, l):
            add('pool', lambda e: e.dma_start(out=Wmem, in_=w_mem_d[l].rearrange("(kc p) n -> p kc n", p=128)),
                writes=['Wmem'], dma=True, dsem='Wmem')
            add('pool', lambda e: e.dma_start(out=Win, in_=w_in_d[l].rearrange("(kc p) n -> p kc n", p=128)),
                writes=['Win'], dma=True, dsem='Win')
            add('pool', lambda e: e.dma_start(out=Wout, in_=w_out_d[l].rearrange("(kc p) n -> p kc n", p=128)),
                writes=['Wout'], dma=True, dsem='Wout')
            add('sp', lambda e: e.dma_start(out=memx, in_=mem_d[s].rearrange("(t p) d -> p t d", p=128)),
                writes=['memx'], dma=True, dsem='memx')
            add('pool', lambda e: e.memset(mvA[:, :, :, 64:65], 1.0), writes=['mvA'])
            for k in range(3):
                add('pool', lambda e, k=k: e.memset(Vaug[k][:, :, 64:65], 1.0), writes=[('Vaug', k)])
            add('pool', lambda e: e.memset(zt[0][:, :, 0:2], 0.0), writes=[('zt', 0)])
            for k in range(3):
                for kv in range(2):
                    add('pool', lambda e, k=k, kv=kv: e.memset(kz[k][kv][:], 0.0), writes=[('kz', k)])
            add('pool', lambda e: e.memset(mkz[:], 0.0), writes=['mkz'])

            for mt in range(2):
                norm_T(memx[:, mt, :], 'memx', gvec[:, GC_MEM + l * 8:GC_MEM + l * 8 + 8],
                       hmT[:, :, mt * 128:(mt + 1) * 128], 'hmT', (0, 1), xn_m, junk_m, 'm')
                for kc in range(8):
                    add('pe', lambda e, kc=kc, mt=mt: e.matmul(PB[2][:], lhsT=hmT[:, kc, mt * 128:(mt + 1) * 128], rhs=Wmem[:, kc, :],
                                                               start=(kc == 0), stop=(kc == 7)),
                        reads=['hmT', 'Wmem'], writes=[('pb', 2)])
                add('act', lambda e: e.activation(out=mks, in_=PB[2][:, 0:256], func=AF.Copy), reads=[('pb', 2)], writes=['mks'])
                add('act', lambda e, mt=mt: e.activation(out=mvA[:, mt, :, 0:64], in_=PB[2][:, 256:512].rearrange("p (h d) -> p h d", h=4), func=AF.Copy),
                    reads=[('pb', 2)], writes=['mvA'])
                add('dve', lambda e: e.tensor_tensor(out=mksq, in0=mks, in1=mks, op=ALU.mult), reads=['mks'], writes=['mksq'])
                add('dve', lambda e: e.reduce_sum(out=mss, in_=mksq.rearrange("p (h d) -> p h d", h=4), axis=AX.X), reads=['mksq'], writes=['mss'])
                add('dve', lambda e: e.tensor_scalar(out=mss2, in0=mss, scalar1=1.0 / 64, scalar2=EPS, op0=ALU.mult, op1=ALU.add),
                    reads=['mss'], writes=['mss2'])
                add('pool', lambda e: e.tensor_tensor(out=mrq, in0=mss2, in1=mhalf[:, 0:4], op=ALU.pow), reads=['mss2', 'mhalf'], writes=['mrq'])
                add('dve', lambda e: e.tensor_tensor(out=mkn.rearrange("p (h d) -> p h d", h=4), in0=mks.rearrange("p (h d) -> p h d", h=4),
                                                     in1=mrq.unsqueeze(2).broadcast_to([128, 4, 64]), op=ALU.mult),
                    reads=['mks', 'mrq'], writes=['mkn'])
                pq = PB[3][:].bitcast(BF16)
                for c in range(2):
                    add('pe', lambda e, c=c, pq=pq: e.transpose(out=pq[:, c * 128:(c + 1) * 128], in_=mkn[:, c * 128:(c + 1) * 128], identity=identb[:]),
                        reads=['mkn', 'identb'], writes=[('pb', 3)])
                for h in range(4):
                    c, hf = h // 2, h % 2
                    lo, hi = hf * 64, hf * 64 + 64
                    add('dve', lambda e, mt=mt, pq=pq, h=h, c=c, lo=lo, hi=hi: e.tensor_scalar(out=mkz[lo:hi, h, mt * 128:(mt + 1) * 128], in0=pq[lo:hi, c * 128:(c + 1) * 128],
                                                                                               scalar1=gvec[lo:hi, GC_QK + l * 8 + 7:GC_QK + l * 8 + 8], scalar2=None, op0=ALU.mult),
                        reads=[('pb', 3), 'gvec'], writes=['mkz'])

            if DEBUG_STAGE == 'mem':
                return
            ntl = NT if DEBUG_STAGE != 'tile1' else 2
            go = GC_OUT + l * 8
            cw = GC_CONV + l * 6

            def stage1(i):
                p = i % 2
                k3 = i % 3
                yield from norm_T_gen(X[:, i, :], ('x', i), gvec[:, GC_MIX + l * 8:GC_MIX + l * 8 + 8], hTt[p], ('hTt', p), (0, 1),
                                      xn_m, ('m', 'xn'), junk_m, ('m', 'junk'))
                for half in range(2):
                    for kc in range(8):
                        add('pe', lambda e, kc=kc, half=half, p=p: e.matmul(PB[2 + half][:], lhsT=hTt[p][:, kc, :], rhs=Win[:, kc, half * 512:(half + 1) * 512],
                                                                          start=(kc == 0), stop=(kc == 7)),
                            reads=[('hTt', p), 'Win'], writes=[('pb', 2 + half)])
                yield
                for cc in range(6):
                    b = cc // 4
                    for kc in range(8):
                        add('pe', lambda e, kc=kc, cc=cc, b=b, p=p: e.matmul(PB[b][:, (cc % 4) * 128:(cc % 4 + 1) * 128],
                                                                           lhsT=Win[:, kc, 1024 + cc * 128:1024 + (cc + 1) * 128], rhs=hTt[p][:, kc, :],
                                                                           start=(kc == 0), stop=(kc == 7)),
                            reads=[('hTt', p), 'Win'], writes=[('pb', b)])
                yield
                add('act', lambda e: e.activation(out=qk[:, 0:512], in_=PB[2][:], func=AF.Copy), reads=[('pb', 2)], writes=['qk'])
                add('act', lambda e: e.activation(out=qk[:, 512:896], in_=PB[3][:, 0:384], func=AF.Copy), reads=[('pb', 3)], writes=['qk'])
                add('act', lambda e, k3=k3: e.activation(out=Vaug[k3][:, :, 0:64], in_=PB[3][:, 384:512].rearrange("p (h d) -> p h d", h=2), func=AF.Copy),
                    reads=[('pb', 3)], writes=[('Vaug', k3)])
                pcb = PB[0][:, 0:256].rearrange("p (c t) -> p c t", c=2)
                pcc = PB[0][:, 256:512].rearrange("p (c t) -> p c t", c=2)
                pcu = PB[1][:, 0:256].rearrange("p (c t) -> p c t", c=2)
                z = zt[p]
                zn = zt[1 - p]
                add('act', lambda e, pcc=pcc: e.activation(out=ccs, in_=pcc, func=AF.Copy), reads=[('pb', 0)], writes=['ccs'])
                yield
                add('act', lambda e: e.activation(out=qsq, in_=qk, func=AF.Square), reads=['qk'], writes=['qsq'])
                yield
                add('dve', lambda e: e.reduce_sum(out=ssq, in_=qsq.rearrange("p (h d) -> p h d", h=14), axis=AX.X), reads=['qsq'], writes=['ssq'])
                yield
                add('pool', lambda e: e.tensor_scalar(out=ssq2, in0=ssq, scalar1=1.0 / 64, scalar2=EPS, op0=ALU.mult, op1=ALU.add),
                    reads=['ssq'], writes=['ssq2'])
                add('pool', lambda e: e.tensor_tensor(out=rq, in0=ssq2, in1=mhalf[:, 0:14], op=ALU.pow), reads=['ssq2', 'mhalf'], writes=['rq'])
                add('dve', lambda e, z=z, pcu=pcu: e.tensor_tensor(out=z[:, :, 2:130], in0=ccs, in1=pcu, op=ALU.mult),
                    reads=['ccs', ('pb', 1)], writes=[('zt', p)])
                yield
                add('pool', lambda e, z=z, zn=zn: e.tensor_copy(out=zn[:, :, 0:2], in_=z[:, :, 128:130]), reads=[('zt', p)], writes=[('zt', 1 - p)])
                add('pool', lambda e: e.tensor_tensor(out=qkn.rearrange("p (h d) -> p h d", h=14), in0=qk.rearrange("p (h d) -> p h d", h=14),
                                                     in1=rq.unsqueeze(2).broadcast_to([128, 14, 64]), op=ALU.mult),
                    reads=['qk', 'rq'], writes=['qkn'])
                yield
                pq = PB[2][:].bitcast(BF16)
                for c in range(7):
                    add('pe', lambda e, c=c, pq=pq: e.transpose(out=pq[:, c * 128:(c + 1) * 128], in_=qkn[:, c * 128:(c + 1) * 128], identity=identb[:]),
                        reads=['qkn', 'identb'], writes=[('pb', 2)])
                for c in range(2):
                    add('dve', lambda e, z=z, c=c: e.tensor_scalar(out=cacc[:, c, :], in0=z[:, c, 0:128], scalar1=gvec[:, cw + c:cw + c + 1], scalar2=None, op0=ALU.mult),
                        reads=[('zt', p), 'gvec'], writes=['cacc'])
                    for j in (1, 2):
                        add('dve', lambda e, z=z, c=c, j=j: e.scalar_tensor_tensor(out=cacc[:, c, :], in0=z[:, c, j:j + 128], scalar=gvec[:, cw + 2 * j + c:cw + 2 * j + c + 1],
                                                                                 in1=cacc[:, c, :], op0=ALU.mult, op1=ALU.add),
                            reads=[('zt', p), 'gvec', 'cacc'], writes=['cacc'])
                yield
                add('dve', lambda e, p=p, pq=pq: e.tensor_tensor(out=qTt[p], in0=pq[:, 0:896].rearrange("p (c t) -> p c t", c=7),
                                                                 in1=gvec[:, GC_QK + l * 8:GC_QK + l * 8 + 7].unsqueeze(2).broadcast_to([128, 7, 128]), op=ALU.mult),
                    reads=[('pb', 2), 'gvec'], writes=[('qTt', p)])
                for kv in range(2):
                    lo, hi = kv * 64, kv * 64 + 64
                    add('dve', lambda e, k3=k3, pq=pq, kv=kv, lo=lo, hi=hi: e.tensor_scalar(out=kz[k3][kv][lo:hi, :], in0=pq[lo:hi, 512:640],
                                                                                            scalar1=gvec[lo:hi, GC_QK + l * 8 + 4:GC_QK + l * 8 + 5], scalar2=None, op0=ALU.mult),
                        reads=[('pb', 2), 'gvec'], writes=[('kz', k3)])
                yield
                add('dve', lambda e, pcb=pcb: e.tensor_tensor(out=ycv, in0=cacc, in1=pcb, op=ALU.mult), reads=['cacc', ('pb', 0)], writes=['ycv'])
                yield
                add('act', lambda e: e.activation(out=csq, in_=ycv, func=AF.Square), reads=['ycv'], writes=['csq'])
                for c in range(2):
                    add('act', lambda e, c=c, p=p: e.activation(out=ycT[p][:, c, :], in_=ycv[:, c, :], func=AF.Copy, scale=gvec[:, go + 4 + c:go + 5 + c]),
                        reads=['ycv', 'gvec'], writes=[('ycT', p)])
                yield
                for c in range(2):
                    add('pe', lambda e, c=c: e.matmul(PB[1][:, 256:257], lhsT=csq[:, c, :], rhs=ones[:, 0:1], start=(c == 0), stop=(c == 1)),
                        reads=['ones', 'csq'], writes=[('pb', 1)])
                yield
                add('dve', lambda e, p=p: e.tensor_scalar(out=rcs[p], in0=PB[1][:, 256:257], scalar1=1.0 / 256, scalar2=EPS, op0=ALU.mult, op1=ALU.add),
                    reads=[('pb', 1)], writes=[('rcs', p)])
                yield
                add('pool', lambda e, p=p: e.tensor_tensor(out=rcinv[p], in0=rcs[p], in1=mhalf[:, 0:1], op=ALU.pow), reads=[('rcs', p), 'mhalf'], writes=[('rcinv', p)])
                yield

            def stage2(i):
                p = i % 2
                k3 = i % 3
                k3p = (i - 1) % 3
                rels = [1] if i == 0 else [0, 1]
                blk = 0
                for kvh in range(2):
                    for r in rels:
                        kk = k3 if r == 1 else k3p
                        b = 4 + blk % 2
                        eb = blk % 2
                        add('pe', lambda e, kk=kk, p=p, kvh=kvh, b=b: e.matmul(PB[b][:], lhsT=kz[kk][kvh][:], rhs=qTt[p][:, 0:4, :], start=True, stop=True),
                            reads=[('kz', kk), ('qTt', p)], writes=[('pb', b)])
                        yield
                        add('act', lambda e, b=b, eb=eb: e.activation(out=es[eb], in_=PB[b][:], func=AF.Exp, scale=0.125),
                            reads=[('pb', b)], writes=[('es', eb)])
                        yield
                        add('dve', lambda e, eb=eb, r=r, kvh=kvh: e.tensor_tensor(out=PT[:, kvh * 2 + r, :].rearrange("p (h q) -> p h q", h=4),
                                                                                  in0=es[eb].rearrange("p (h q) -> p h q", h=4),
                                                                                  in1=ET[:, r, kvh * 4:kvh * 4 + 4, :], op=ALU.mult),
                            reads=[('es', eb), 'ET'], writes=[('PT', kvh * 2 + r)])
                        blk += 1
                yield
                for h in range(8):
                    kvh, g = h // 4, h % 4
                    b = 6 + kvh
                    po = PB[b][:, 0:260].rearrange("p (h d) -> p h d", h=4)
                    for ri, r in enumerate(rels):
                        kk = k3 if r == 1 else k3p
                        lastr = (ri == len(rels) - 1)
                        add('pe', lambda e, po=po, g=g, kvh=kvh, r=r, kk=kk, ri=ri, lastr=lastr: e.matmul(po[:, g, :], lhsT=PT[:, kvh * 2 + r, g * 128:(g + 1) * 128],
                                                                                                        rhs=Vaug[kk][:, kvh, :], start=(ri == 0), stop=lastr),
                            reads=[('PT', kvh * 2 + r), ('Vaug', kk)], writes=[('pb', b)])
                yield
                for mt in range(2):
                    b = 4 + mt
                    for h in range(4):
                        c = h // 2
                        add('pe', lambda e, b=b, h=h, c=c, mt=mt, p=p: e.matmul(PB[b][:, h * 128:(h + 1) * 128], lhsT=mkz[:, h, mt * 128:(mt + 1) * 128],
                                                                              rhs=qTt[p][:, 5 + c, :], start=True, stop=True),
                            reads=['mkz', ('qTt', p)], writes=[('pb', b)])
                    yield
                    add('act', lambda e, b=b, mt=mt: e.activation(out=PmT[:, mt, :], in_=PB[b][:], func=AF.Exp, scale=0.125),
                        reads=[('pb', b)], writes=[('PmT', mt)])
                for kvh in range(2):
                    po = PB[6 + kvh][:, 0:260].rearrange("p (h d) -> p h d", h=4)
                    add('dve', lambda e, po=po, kvh=kvh: e.tensor_tensor(out=den[:, kvh * 4:kvh * 4 + 4], in0=po[:, :, 64],
                                                                         in1=esink[:, l * 8 + kvh * 4:l * 8 + kvh * 4 + 4], op=ALU.add),
                        reads=[('pb', 6 + kvh), 'esink'], writes=['den'])
                yield
                add('dve', lambda e: e.reciprocal(out=rden, in_=den), reads=['den'], writes=['rden'])
                yield
                for kvh in range(2):
                    po = PB[6 + kvh][:, 0:260].rearrange("p (h d) -> p h d", h=4)
                    add('dve', lambda e, po=po, kvh=kvh: e.tensor_tensor(out=ysw[:, kvh * 256:(kvh + 1) * 256].rearrange("p (h d) -> p h d", h=4), in0=po[:, :, 0:64],
                                                                         in1=rden[:, kvh * 4:kvh * 4 + 4].unsqueeze(2).broadcast_to([128, 4, 64]), op=ALU.mult),
                        reads=[('pb', 6 + kvh), 'rden'], writes=['ysw'])
                pom = PB[4][:, 0:260].rearrange("p (h d) -> p h d", h=4)
                for h in range(4):
                    for mt in range(2):
                        add('pe', lambda e, h=h, mt=mt, pom=pom: e.matmul(pom[:, h, :], lhsT=PmT[:, mt, h * 128:(h + 1) * 128], rhs=mvA[:, mt, h, :],
                                                                         start=(mt == 0), stop=(mt == 1)),
                            reads=[('PmT', mt), 'mvA'], writes=[('pb', 4)])
                yield
                add('act', lambda e: e.activation(out=junk2[:, 0:512], in_=ysw, func=AF.Square, accum_out=oss[:, 0:1]),
                    reads=['ysw'], writes=['junk2', 'oss'])
                add('dve', lambda e, pom=pom: e.reciprocal(out=rdm, in_=pom[:, :, 64]), reads=[('pb', 4)], writes=['rdm'])
                yield
                add('dve', lambda e, pom=pom: e.tensor_tensor(out=ym.rearrange("p (h d) -> p h d", h=4), in0=pom[:, :, 0:64],
                                                              in1=rdm.unsqueeze(2).broadcast_to([128, 4, 64]), op=ALU.mult),
                    reads=[('pb', 4), 'rdm'], writes=['ym'])
                yield
                add('act', lambda e: e.activation(out=junk2[:, 512:768], in_=ym, func=AF.Square, accum_out=oss[:, 1:2]),
                    reads=['ym'], writes=['junk2', 'oss'])
                yield
                add('pool', lambda e: e.tensor_tensor(out=oss2, in0=oss, in1=oscale, op=ALU.mult), reads=['oss', 'oscale'], writes=['oss2'])
                add('pool', lambda e: e.tensor_scalar(out=oss3, in0=oss2, scalar1=1.0, scalar2=EPS, op0=ALU.mult, op1=ALU.add), reads=['oss2'], writes=['oss3'])
                add('pool', lambda e: e.tensor_tensor(out=orin, in0=oss3, in1=mhalf[:, 0:2], op=ALU.pow), reads=['oss3', 'mhalf'], writes=['orin'])
                yield
                add('act', lambda e: e.activation(out=ynb[:, 0:512], in_=ysw, func=AF.Copy, scale=orin[:, 0:1]), reads=['ysw', 'orin'], writes=['ynb'])
                add('act', lambda e: e.activation(out=ynb[:, 512:768], in_=ym, func=AF.Copy, scale=orin[:, 1:2]), reads=['ym', 'orin'], writes=['ynb'])
                yield
                pyt = PB[5][:].bitcast(BF16)
                for c in range(6):
                    add('pe', lambda e, c=c, pyt=pyt: e.transpose(out=pyt[:, c * 128:(c + 1) * 128], in_=ynb[:, c * 128:(c + 1) * 128], identity=identb[:]),
                        reads=['ynb', 'identb'], writes=[('pb', 5)])
                yield
                add('dve', lambda e, pyt=pyt: e.tensor_tensor(out=yT[:, 0:4, :], in0=pyt[:, 0:512].rearrange("p (c t) -> p c t", c=4),
                                                              in1=gvec[:, go:go + 4].unsqueeze(2).broadcast_to([128, 4, 128]), op=ALU.mult),
                    reads=[('pb', 5), 'gvec'], writes=['yT'])
                add('dve', lambda e, pyt=pyt: e.tensor_tensor(out=yT[:, 6:8, :], in0=pyt[:, 512:768].rearrange("p (c t) -> p c t", c=2),
                                                              in1=gvec[:, go + 6:go + 8].unsqueeze(2).broadcast_to([128, 2, 128]), op=ALU.mult),
                    reads=[('pb', 5), 'gvec'], writes=['yT'])
                yield
                for half in range(2):
                    b = 6 + half
                    main = [0, 1, 2, 3, 6, 7]
                    for n_, kc in enumerate(main):
                        add('pe', lambda e, kc=kc, half=half, b=b, n_=n_: e.matmul(PB[b][:], lhsT=yT[:, kc, :], rhs=Wout[:, kc, half * 512:(half + 1) * 512],
                                                                                 start=(n_ == 0), stop=(n_ == 5)),
                            reads=['yT', 'Wout'], writes=[('pb', b)])
                    bc_ = 4 + half
                    for c in range(2):
                        add('pe', lambda e, c=c, half=half, bc_=bc_, p=p: e.matmul(PB[bc_][:], lhsT=ycT[p][:, c, :], rhs=Wout[:, 4 + c, half * 512:(half + 1) * 512],
                                                                                 start=(c == 0), stop=(c == 1)),
                            reads=[('ycT', p), 'Wout'], writes=[('pb', bc_)])
                    yield
                    xs = X[:, i, half * 512:(half + 1) * 512]
                    add('dve', lambda e, b=b, xs=xs: e.tensor_tensor(out=xs, in0=PB[b][:], in1=xs, op=ALU.add),
                        reads=[('pb', b), ('x', i)], writes=[('x', i)])
                    add('dve', lambda e, bc_=bc_, xs=xs, p=p: e.scalar_tensor_tensor(out=xs, in0=PB[bc_][:], scalar=rcinv[p], in1=xs, op0=ALU.mult, op1=ALU.add),
                        reads=[('pb', bc_), ('x', i), ('rcinv', p)], writes=[('x', i)])
                yield

            for step in range(ntl + 1):
                gens = []
                if step < ntl:
                    gens.append(stage1(step))
                if step >= 1:
                    gens.append(stage2(step - 1))
                round_robin(gens)

        gcount = [0]

        def ffn(s, l):
            moe = (l % 2 == 1)
            li = l // 2
            experts = list(range(N_EXP)) if moe else [None]
            groups = [(ex, g) for ex in experts for g in range(NG)]

            def issue_dma(ex, g):
                gi = gcount[0]
                gcount[0] += 1
                b = gi % NWB
                Wg_, Wu_, Wd_ = Wb[b]
                if ex is None:
                    sg_, su_, sd_ = wgd_d[li], wud_d[li], wdd_d[li]
                else:
                    sg_, su_, sd_ = wgm_d[li, ex], wum_d[li, ex], wdm_d[li, ex]
                add('pool', lambda e: e.dma_start(out=Wg_, in_=sg_[:, g * GW:(g + 1) * GW].rearrange("(kc p) n -> p kc n", p=128)),
                    writes=[('Wg', b)], dma=True, dsem=('Wg', b))
                add('pool', lambda e: e.dma_start(out=Wu_, in_=su_[:, g * GW:(g + 1) * GW].rearrange("(kc p) n -> p kc n", p=128)),
                    writes=[('Wu', b)], dma=True, dsem=('Wu', b))
                add('pool', lambda e: e.dma_start(out=Wd_, in_=sd_[g * GW:(g + 1) * GW, :].rearrange("(fc p) n -> p fc n", p=128)),
                    writes=[('Wd', b)], dma=True, dsem=('Wd', b))
                return b

            bufs = {}
            PRE = NWB - 1
            for k in range(min(PRE, len(groups))):
                bufs[k] = issue_dma(*groups[k])
            xn_par = [xn_f, AT[1].rearrange("p a b -> p (a b)").bitcast(F32)]
            xn_res = [('f', 'xn'), ('AT', 1)]
            jflat = AT[0].rearrange("p a b -> p (a b)")
            junk_par = [jflat[:, 0:1024], jflat[:, 1024:2048]]

            def ffn_norm_tile(i):
                k = i % 2
                yield from norm_T_gen(X[:, i, :], ('x', i), gvec[:, GC_FFN + l * 8:GC_FFN + l * 8 + 8], hT[:, :, i * 128:(i + 1) * 128], 'hT',
                                      (2 * k, 2 * k + 1), xn_par[k], xn_res[k], junk_par[k], ('AT', 0),
                                      f32dst=(h2f[k] if moe else None), f32res=('h2f', k))
                if moe:
                    for kc in range(8):
                        add('pe', lambda e, kc=kc, k=k: e.matmul(PB[4 + k][:, 0:8], lhsT=h2f[k][:, kc, :], rhs=wr[:, li, kc, :], start=(kc == 0), stop=(kc == 7)),
                            reads=[('h2f', k), 'wr'], writes=[('pb', 4 + k)])
                    yield
                    add('dve', lambda e, i=i, k=k: e.tensor_tensor(out=L[:, i, :], in0=PB[4 + k][:, 0:8], in1=brow[:, BR_BR + li * 8:BR_BR + li * 8 + 8], op=ALU.add),
                        reads=[('pb', 4 + k), 'brow'], writes=['L'])
                    yield

            for i in range(0, NT, 2):
                round_robin([ffn_norm_tile(i), ffn_norm_tile(i + 1)])
            if moe:
                bc = lambda a: a.unsqueeze(2).broadcast_to([128, NT, 8])
                add('dve', lambda e: e.reduce_max(out=m1, in_=L, axis=AX.X), reads=['L'], writes=['m1'])
                add('dve', lambda e: e.tensor_tensor(out=eq1, in0=L, in1=bc(m1), op=ALU.is_equal), reads=['L', 'm1'], writes=['eq1'])
                add('dve', lambda e: e.scalar_tensor_tensor(out=L2.rearrange("p a b -> p (a b)"), in0=eq1.rearrange("p a b -> p (a b)"), scalar=-1e30,
                                                            in1=L.rearrange("p a b -> p (a b)"), op0=ALU.mult, op1=ALU.add),
                    reads=['eq1', 'L'], writes=['L2'])
                add('dve', lambda e: e.reduce_max(out=m2, in_=L2, axis=AX.X), reads=['L2'], writes=['m2'])
                add('dve', lambda e: e.tensor_tensor(out=eq2, in0=L2, in1=bc(m2), op=ALU.is_equal), reads=['L2', 'm2'], writes=['eq2'])
                add('dve', lambda e: e.tensor_tensor(out=dd, in0=m2, in1=m1, op=ALU.subtract), reads=['m1', 'm2'], writes=['dd'])
                add('act', lambda e: e.activation(out=ee, in_=dd, func=AF.Exp), reads=['dd'], writes=['ee'])
                add('dve', lambda e: e.tensor_scalar(out=g1, in0=ee, scalar1=1.0, scalar2=None, op0=ALU.add), reads=['ee'], writes=['g1'])
                add('dve', lambda e: e.reciprocal(out=g1, in_=g1), reads=['g1'], writes=['g1'])
                add('dve', lambda e: e.tensor_tensor(out=g2, in0=ee, in1=g1, op=ALU.mult), reads=['ee', 'g1'], writes=['g2'])
                add('dve', lambda e: e.tensor_tensor(out=eq1, in0=eq1, in1=bc(g1), op=ALU.mult), reads=['eq1', 'g1'], writes=['eq1'])
                add('dve', lambda e: e.tensor_tensor(out=eq2, in0=eq2, in1=bc(g2), op=ALU.mult), reads=['eq2', 'g2'], writes=['eq2'])
                add('dve', lambda e: e.tensor_tensor(out=comb, in0=eq1, in1=eq2, op=ALU.add), reads=['eq1', 'eq2'], writes=['comb'])

            pending = [None]
            step = [0]
            dcount = [0]

            def do_down(ex, b, tg, ap):
                Wd_ = Wb[b][2]
                for tt in range(4):
                    tile = tg * 4 + tt
                    for half in range(2):
                        db = 4 + dcount[0] % 4
                        dcount[0] += 1
                        for fcl in range(GW // 128):
                            add('pe', lambda e, db=db, fcl=fcl, tt=tt, half=half: e.matmul(PB[db][:], lhsT=AT[ap][:, fcl, tt * 128:(tt + 1) * 128],
                                                                                         rhs=Wd_[:, fcl, half * 512:(half + 1) * 512],
                                                                                         start=(fcl == 0), stop=(fcl == GW // 128 - 1)),
                                reads=[('AT', ap), ('Wd', b)], writes=[('pb', db)])
                        xs = X[:, tile, half * 512:(half + 1) * 512]
                        if ex is None:
                            add('dve', lambda e, db=db, xs=xs: e.tensor_tensor(out=xs, in0=PB[db][:], in1=xs, op=ALU.add),
                                reads=[('pb', db), ('x', tile)], writes=[('x', tile)])
                        else:
                            add('dve', lambda e, db=db, xs=xs, tile=tile: e.scalar_tensor_tensor(out=xs, in0=PB[db][:], scalar=comb[:, tile, ex:ex + 1], in1=xs,
                                                                                               op0=ALU.mult, op1=ALU.add),
                                reads=[('pb', db), ('x', tile), 'comb'], writes=[('x', tile)])

            for k, (ex, g) in enumerate(groups):
                b = bufs[k]
                Wg_, Wu_, _ = Wb[b]
                for tg in range(4):
                    ap = step[0] % 2
                    step[0] += 1
                    for fcl in range(GW // 128):
                        gb = fcl % 2
                        ub = 2 + fcl % 2
                        for kc in range(8):
                            add('pe', lambda e, kc=kc, fcl=fcl, gb=gb, tg=tg, Wg_=Wg_: e.matmul(PB[gb][:], lhsT=Wg_[:, kc, fcl * 128:(fcl + 1) * 128], rhs=hT[:, kc, tg * 512:(tg + 1) * 512],
                                                                                     start=(kc == 0), stop=(kc == 7)),
                                reads=[('Wg', b), 'hT'], writes=[('pb', gb)])
                        for kc in range(8):
                            add('pe', lambda e, kc=kc, fcl=fcl, ub=ub, tg=tg, Wu_=Wu_: e.matmul(PB[ub][:], lhsT=Wu_[:, kc, fcl * 128:(fcl + 1) * 128], rhs=hT[:, kc, tg * 512:(tg + 1) * 512],
                                                                                     start=(kc == 0), stop=(kc == 7)),
                                reads=[('Wu', b), 'hT'], writes=[('pb', ub)])
                        add('act', lambda e, gb=gb: e.activation(out=sg[gb], in_=PB[gb][:], func=AF.Silu), reads=[('pb', gb)], writes=[('sg', gb)])
                        add('dve', lambda e, gb=gb, ub=ub, fcl=fcl, ap=ap: e.tensor_tensor(out=AT[ap][:, fcl, :], in0=sg[gb], in1=PB[ub][:], op=ALU.mult),
                            reads=[('sg', gb), ('pb', ub)], writes=[('AT', ap)])
                    if pending[0] is not None:
                        do_down(*pending[0])
                    pending[0] = (ex, b, tg, ap)
                    if tg == 0 and k + PRE < len(groups):
                        bufs[k + PRE] = issue_dma(*groups[k + PRE])
            do_down(*pending[0])

        for s in range(n_seq):
            for q4 in range(4):
                add('sp', lambda e, q4=q4, s=s: e.dma_start(out=X[:, q4 * 4:(q4 + 1) * 4, :], in_=x_d[s, q4 * 512:(q4 + 1) * 512, :].rearrange("(t p) d -> p t d", p=128)),
                    writes=[('x', q4 * 4 + t) for t in range(4)], dma=True, dsem=('xin', q4))
            for l in layers:
                S.barrier()
                mixer(s, l)
                S.barrier()
                if DEBUG_STAGE in ('mem', 'tile1', 'mixer'):
                    continue
                ffn(s, l)
            for q4 in range(4):
                add('sp', lambda e, q4=q4, s=s: e.dma_start(out=out_d[s, q4 * 512:(q4 + 1) * 512, :].rearrange("(t p) d -> p t d", p=128), in_=X[:, q4 * 4:(q4 + 1) * 4, :]),
                    reads=[('x', q4 * 4 + t) for t in range(4)], dma=True, dsem=('xout', q4))
        S.emit_all()
    return nc


def _alibi_table():
    j = np.arange(128)[:, None, None, None].astype(np.float64)
    r = np.arange(2)[None, :, None, None]
    h = np.arange(8)[None, None, :, None]
    i = np.arange(128)[None, None, None, :].astype(np.float64)
    kpos = j + (r - 1) * 128
    kc = np.floor(kpos / 64)
    qc = np.floor(i / 64)
    valid = (kc >= qc - 2) & (kc <= qc)
    slope = 2.0 ** (-(h + 1.0))
    t = np.where(valid, np.exp(-slope * np.abs(i - kpos)), 0.0)
    return np.ascontiguousarray(t.reshape(128, 2 * 8 * 128)).astype(np.float32)


def _prep_small(inp):
    f = lambda a: np.asarray(a, dtype=np.float32)
    gvec = np.zeros((128, GC_N), np.float32)
    g_out = np.concatenate([f(inp['g_out_swa']), f(inp['g_out_conv']), f(inp['g_out_mem'])], axis=1)
    for l in range(DEPTH):
        gvec[:, GC_MIX + l * 8:GC_MIX + l * 8 + 8] = f(inp['g_mix'])[l].reshape(8, 128).T
        gvec[:, GC_FFN + l * 8:GC_FFN + l * 8 + 8] = f(inp['g_ffn'])[l].reshape(8, 128).T
        gvec[:, GC_MEM + l * 8:GC_MEM + l * 8 + 8] = f(inp['g_mem'])[l].reshape(8, 128).T
        gvec[:, GC_OUT + l * 8:GC_OUT + l * 8 + 8] = g_out[l].reshape(8, 128).T
        gq = np.tile(f(inp['g_q_swa'])[l], 2)
        gk = np.tile(f(inp['g_k_swa'])[l], 2)
        gqm = np.tile(f(inp['g_q_mem'])[l], 2)
        gkm = np.tile(f(inp['g_k_mem'])[l], 2)
        for c in range(4):
            gvec[:, GC_QK + l * 8 + c] = gq
        gvec[:, GC_QK + l * 8 + 4] = gk
        gvec[:, GC_QK + l * 8 + 5] = gqm
        gvec[:, GC_QK + l * 8 + 6] = gqm
        gvec[:, GC_QK + l * 8 + 7] = gkm
        cw = f(inp['conv_w'])[l]
        for j in range(3):
            for c in range(2):
                gvec[:, GC_CONV + l * 6 + 2 * j + c] = cw[j, c * 128:(c + 1) * 128]
    brow = np.zeros((128, BR_N), np.float32)
    brow[:, BR_SINK:BR_SINK + 32] = f(inp['sinks']).reshape(1, 32)
    brow[:, BR_BR:BR_BR + 16] = f(inp['b_router']).reshape(1, 16)
    wrr = f(inp['w_router']).reshape(2, 8, 128, 8).transpose(2, 0, 1, 3).reshape(128, 128)
    return gvec, brow, np.ascontiguousarray(wrr)


def _perm_w_in(w_in):
    heads = [0, 4, 1, 5, 2, 6, 3, 7]
    cols = []
    for h in heads:
        cols += list(range(h * 64, (h + 1) * 64))
    cols += list(range(512, 640))
    cols += list(range(1536, 1792))
    cols += list(range(640, 768))
    cols += list(range(768, 1536))
    return np.ascontiguousarray(np.asarray(w_in, dtype=np.float32)[:, :, cols])


def kernel(**inp):
    n_cores = 8
    x = np.asarray(inp['x'], dtype=np.float32)
    mem = np.asarray(inp['mem'], dtype=np.float32)
    B = x.shape[0]
    n_seq = B // n_cores
    gvec, brow, wrr = _prep_small(inp)
    shared = {
        "w_in": _perm_w_in(inp['w_in']),
        "w_mem_kv": np.asarray(inp['w_mem_kv'], dtype=np.float32),
        "w_out": np.asarray(inp['w_out'], dtype=np.float32),
        "w_gate_dense": np.asarray(inp['w_gate_dense'], dtype=np.float32),
        "w_up_dense": np.asarray(inp['w_up_dense'], dtype=np.float32),
        "w_down_dense": np.asarray(inp['w_down_dense'], dtype=np.float32),
        "w_gate_moe": np.asarray(inp['w_gate_moe'], dtype=np.float32),
        "w_up_moe": np.asarray(inp['w_up_moe'], dtype=np.float32),
        "w_down_moe": np.asarray(inp['w_down_moe'], dtype=np.float32),
        "gvec": gvec, "brow": brow, "wr": wrr, "et": _alibi_table(),
        "ident": np.eye(128, dtype=np.float32),
    }
    nc = build_program(layers=tuple(range(DEPTH)), n_seq=n_seq)
    in_maps = []
    for c in range(n_cores):
        m = dict(shared)
        m["x"] = np.ascontiguousarray(x[c * n_seq:(c + 1) * n_seq])
        m["mem"] = np.ascontiguousarray(mem[c * n_seq:(c + 1) * n_seq])
        in_maps.append(m)
    res = run_bass_kernel_spmd(nc, in_maps, core_ids=list(range(n_cores)))
    out = np.concatenate([np.asarray(r["out"]) for r in res.results], axis=0)
    return out.astype(np.float32)
```
